# Optimizing a Trainium2 kernel written in Bass

```python
import jax, jax.numpy as jnp
from jax import lax
import numpy as np

D_MODEL = 2048
BATCH = 4
SEQ = 4096
DEPTH = 4

GRID_W = 64
CTX_LEN = 256

N_MIXERS = 3
N_GLA_LAYERS = (DEPTH + 2) // 3
N_SWA_LAYERS = (DEPTH + 1) // 3
N_CONV_LAYERS = DEPTH // 3

GLA_HEADS = 4
GLA_KEY_WIDTH = D_MODEL // 2
GLA_VALUE_WIDTH = D_MODEL
GLA_HEAD_V = GLA_VALUE_WIDTH // GLA_HEADS
GLA_GATE_RANK = 16
GLA_GATE_NORM = 16.0
GLA_CHUNK = 64

SWA_HEAD_DIM = 64
SWA_Q_HEADS = D_MODEL // SWA_HEAD_DIM
SWA_KV_HEADS = SWA_Q_HEADS // 8
SWA_GROUP = SWA_Q_HEADS // SWA_KV_HEADS
SWA_Q_WIDTH = SWA_Q_HEADS * SWA_HEAD_DIM
SWA_KV_WIDTH = SWA_KV_HEADS * SWA_HEAD_DIM
WINDOW = 128
WIN_BLOCK = 128
BAND = WIN_BLOCK + 2 * WINDOW
ROPE_BASE = 10000.0
ROPE_AXIS_FREQS = SWA_HEAD_DIM // 4

CONV_WIDTH = 3

N_EXPERTS = 32
TOP_K = 4
EXPERT_FF = 3 * D_MODEL // 8
SWIGLU_LIMIT = 7.0
SWIGLU_ALPHA = 1.702
MOE_ROW_BLOCK = 256

N_MOD = 6
LN_EPS = 1e-5
DEEPNORM_ALPHA = (2 * DEPTH) ** 0.25
DEEPNORM_BETA = (8 * DEPTH) ** -0.25
NEG_INF = -1e30

kernel_name = 'hybrid_gla_swa_shortconv_moe_dit'

F32 = jnp.float32


def _layer_norm(x, g, b):
    xf = x.astype(F32)
    xc = xf - jnp.mean(xf, -1, keepdims=True)
    var = jnp.mean(xc * xc, -1, keepdims=True)
    return (xc * lax.rsqrt(var + LN_EPS) * g.astype(F32) + b.astype(F32)).astype(x.dtype)


def _split_apply(t, n_ctx, f_ctx, f_lat):
    return jnp.concatenate([f_ctx(t[:, :n_ctx]), f_lat(t[:, n_ctx:])], axis=1)


def _reverse_segments(t, n_ctx):
    return _split_apply(t, n_ctx, lambda s: jnp.flip(s, 1), lambda s: jnp.flip(s, 1))


def _axial_rope_tables(rows):
    row = jnp.repeat(jnp.arange(rows, dtype=jnp.int32), GRID_W).astype(F32)
    col = jnp.tile(jnp.arange(GRID_W, dtype=jnp.int32), rows).astype(F32)
    inv_freq = ROPE_BASE ** (-jnp.arange(ROPE_AXIS_FREQS, dtype=F32) / ROPE_AXIS_FREQS)
    ang_r = row[:, None] * inv_freq
    ang_c = col[:, None] * inv_freq
    return (jnp.cos(ang_r), jnp.sin(ang_r), jnp.cos(ang_c), jnp.sin(ang_c))


def _rotate(xp, cos, sin):
    x1, x2 = jnp.split(xp, 2, axis=-1)
    cos = cos[None, :, None, :].astype(xp.dtype)
    sin = sin[None, :, None, :].astype(xp.dtype)
    return jnp.concatenate([x1 * cos - x2 * sin, x2 * cos + x1 * sin], axis=-1)


def _axial_rope(x, tables):
    cos_r, sin_r, cos_c, sin_c = tables
    x_row, x_col = jnp.split(x, 2, axis=-1)
    return jnp.concatenate([_rotate(x_row, cos_r, sin_r), _rotate(x_col, cos_c, sin_c)], axis=-1)


def _gla_chunked(q, k, v, log_a):
    bsz, length, heads, dk = q.shape
    n_chunks = length // GLA_CHUNK

    def chunks(t):
        return t.astype(F32).reshape(bsz, n_chunks, GLA_CHUNK, heads, t.shape[-1]).transpose(1, 0, 3, 2, 4)

    qc = chunks(q) * (dk ** -0.5)
    kc, vc, ac = chunks(k), chunks(v), chunks(log_a)
    cum = jnp.cumsum(ac, axis=3)
    total = cum[:, :, :, -1:, :]
    q_dec = qc * jnp.exp(cum)
    k_intra = kc * jnp.exp(-cum)
    k_state = kc * jnp.exp(total - cum)
    lower = jnp.tril(jnp.ones((GLA_CHUNK, GLA_CHUNK), dtype=bool))
    scores = jnp.where(lower, jnp.einsum('nbhcd,nbhsd->nbhcs', q_dec, k_intra), 0.0)
    o_intra = jnp.einsum('nbhcs,nbhse->nbhce', scores, vc)

    def step(state, xs):
        qd, ks, vv, decay = xs
        out = jnp.einsum('bhcd,bhde->bhce', qd, state)
        state = state * decay[:, :, 0, :, None] + jnp.einsum('bhcd,bhce->bhde', ks, vv)
        return state, out

    init = jnp.zeros((bsz, heads, dk, vc.shape[-1]), F32)
    _, o_inter = lax.scan(step, init, (q_dec, k_state, vc, jnp.exp(total)))
    o = o_intra + o_inter
    return o.transpose(1, 0, 3, 2, 4).reshape(bsz, length, heads, -1)


def _gla_mixer(u, n_ctx, w_in, gate_w1, gate_w2, gate_b, norm_g, w_out):
    bsz, length, _ = u.shape
    q, k, v, g = jnp.split(u @ w_in, [GLA_KEY_WIDTH, 2 * GLA_KEY_WIDTH, 2 * GLA_KEY_WIDTH + GLA_VALUE_WIDTH], axis=-1)
    heads = lambda t: t.reshape(bsz, length, GLA_HEADS, -1)
    q, k, v = heads(q), heads(k), heads(v)

    def log_decay(d):
        z = (u @ gate_w1[d]) @ gate_w2[d] + gate_b[d]
        return heads(jax.nn.log_sigmoid(z.astype(F32)) / GLA_GATE_NORM)

    rev = lambda t: _reverse_segments(t, n_ctx)
    o_fwd = _gla_chunked(q, k, v, log_decay(0))
    o_bwd = rev(_gla_chunked(rev(q), rev(k), rev(v), rev(log_decay(1))))
    o = o_fwd + o_bwd
    o = o * lax.rsqrt(jnp.mean(o * o, -1, keepdims=True) + LN_EPS) * norm_g.astype(F32)
    o = o * jax.nn.silu(heads(g).astype(F32))
    return o.reshape(bsz, length, GLA_VALUE_WIDTH).astype(u.dtype) @ w_out


def _sink_attention(q, k, v, sink, mask):
    s = jnp.einsum('bqhgd,bkhd->bhgqk', q, k).astype(F32) * (SWA_HEAD_DIM ** -0.5)
    if mask is not None:
        s = jnp.where(mask, s, NEG_INF)
    sink_col = jnp.broadcast_to(sink.astype(F32)[None, :, :, None, None], s.shape[:-1] + (1,))
    p = jax.nn.softmax(jnp.concatenate([s, sink_col], axis=-1), axis=-1)[..., :-1]
    return jnp.einsum('bhgqk,bkhd->bqhgd', p.astype(v.dtype), v)


def _window_mixer(u, n_ctx, w_qkv, b_qkv, sink, w_out, b_out, rope):
    bsz, length, _ = u.shape
    q, k, v = jnp.split(u @ w_qkv + b_qkv, [SWA_Q_WIDTH, SWA_Q_WIDTH + SWA_KV_WIDTH], axis=-1)
    q = q.reshape(bsz, length, SWA_Q_HEADS, SWA_HEAD_DIM)
    k = k.reshape(bsz, length, SWA_KV_HEADS, SWA_HEAD_DIM)
    v = v.reshape(bsz, length, SWA_KV_HEADS, SWA_HEAD_DIM)
    sink = sink.reshape(SWA_KV_HEADS, SWA_GROUP)
    grp = lambda t: t.reshape(t.shape[0], t.shape[1], SWA_KV_HEADS, SWA_GROUP, SWA_HEAD_DIM)

    qc, kc, vc = grp(q[:, :n_ctx]), k[:, :n_ctx], v[:, :n_ctx]
    o_ctx = _sink_attention(qc, kc, vc, sink, None).reshape(bsz, n_ctx, SWA_Q_WIDTH)

    ql = grp(_axial_rope(q[:, n_ctx:], rope))
    kl = _axial_rope(k[:, n_ctx:], rope)
    vl = v[:, n_ctx:]
    seq = ql.shape[1]
    n_blocks = seq // WIN_BLOCK
    k_pad = jnp.pad(kl, ((0, 0), (WINDOW, WINDOW), (0, 0), (0, 0)))
    v_pad = jnp.pad(vl, ((0, 0), (WINDOW, WINDOW), (0, 0), (0, 0)))
    q_blocks = ql.reshape(bsz, n_blocks, WIN_BLOCK, SWA_KV_HEADS, SWA_GROUP, SWA_HEAD_DIM).transpose(1, 0, 2, 3, 4, 5)
    offs = jnp.arange(BAND)
    band = jnp.abs(offs[None, :] - WINDOW - jnp.arange(WIN_BLOCK)[:, None]) <= WINDOW

    def block(args):
        n, qb = args
        start = n * WIN_BLOCK
        kb = lax.dynamic_slice_in_dim(k_pad, start, BAND, axis=1)
        vb = lax.dynamic_slice_in_dim(v_pad, start, BAND, axis=1)
        key_pos = start - WINDOW + offs
        valid = band & ((key_pos >= 0) & (key_pos < seq))[None, :]
        mask = jnp.concatenate([jnp.ones((WIN_BLOCK, n_ctx), dtype=bool), valid], axis=1)
        return _sink_attention(qb, jnp.concatenate([kc, kb], 1), jnp.concatenate([vc, vb], 1), sink, mask)

    o_lat = lax.map(block, (jnp.arange(n_blocks), q_blocks))
    o_lat = o_lat.transpose(1, 0, 2, 3, 4, 5).reshape(bsz, seq, SWA_Q_WIDTH)
    return jnp.concatenate([o_ctx, o_lat], axis=1) @ w_out + b_out


def _depthwise_conv(z, w):
    return lax.conv_general_dilated(z, w.astype(z.dtype)[:, None, :], window_strides=(1,),
                                    padding=((CONV_WIDTH // 2, CONV_WIDTH // 2),),
                                    dimension_numbers=('NWC', 'WIO', 'NWC'),
                                    feature_group_count=z.shape[-1])


def _conv_mixer(u, n_ctx, w_in, w_conv, w_out):
    gate_in, gate_out, val = jnp.split(u @ w_in, 3, axis=-1)
    z = _split_apply(gate_in * val, n_ctx, lambda s: _depthwise_conv(s, w_conv), lambda s: _depthwise_conv(s, w_conv))
    return (gate_out * z) @ w_out


def _moe(u, w_router, b_router, w_gate_up, b_gate_up, w_down, b_down):
    bsz, length, d = u.shape
    xt = u.reshape(-1, d)
    n_tok = xt.shape[0]
    logits = (xt @ w_router + b_router).astype(F32)
    top_val, top_exp = lax.top_k(logits, TOP_K)
    gates = jax.nn.softmax(top_val, axis=-1)
    n_assign = n_tok * TOP_K
    flat_e = top_exp.reshape(-1)
    flat_t = jnp.repeat(jnp.arange(n_tok, dtype=jnp.int32), TOP_K)
    order = jnp.argsort(flat_e)
    e_s, t_s, g_s = flat_e[order], flat_t[order], gates.reshape(-1)[order]
    counts = jnp.bincount(flat_e, length=N_EXPERTS)
    padded = (counts + MOE_ROW_BLOCK - 1) // MOE_ROW_BLOCK * MOE_ROW_BLOCK
    start = jnp.cumsum(counts) - counts
    pad_end = jnp.cumsum(padded)
    pad_start = pad_end - padded
    dest = pad_start[e_s] + jnp.arange(n_assign) - start[e_s]
    n_blocks = -(-n_assign // MOE_ROW_BLOCK) + N_EXPERTS
    n_rows = n_blocks * MOE_ROW_BLOCK
    row_tok = jnp.zeros((n_rows,), jnp.int32).at[dest].set(t_s)
    row_gate = jnp.zeros((n_rows,), xt.dtype).at[dest].set(g_s.astype(xt.dtype))
    blk_exp = jnp.minimum(jnp.searchsorted(pad_end, jnp.arange(n_blocks) * MOE_ROW_BLOCK, side='right'), N_EXPERTS - 1)

    def expert_block(args):
        tok, gw, e = args
        gu = xt[tok] @ w_gate_up[e] + b_gate_up[e]
        glu, lin = jnp.split(gu, 2, axis=-1)
        glu = jnp.minimum(glu, SWIGLU_LIMIT)
        lin = jnp.clip(lin, -SWIGLU_LIMIT, SWIGLU_LIMIT)
        act = glu * jax.nn.sigmoid(SWIGLU_ALPHA * glu) * (lin + 1.0)
        return (act @ w_down[e] + b_down[e]) * gw[:, None]

    y_rows = lax.map(expert_block, (row_tok.reshape(n_blocks, MOE_ROW_BLOCK),
                                    row_gate.reshape(n_blocks, MOE_ROW_BLOCK), blk_exp))
    y = jax.ops.segment_sum(y_rows.reshape(n_rows, d), row_tok, num_segments=n_tok)
    return y.reshape(bsz, length, d)


def setup_inputs(seed: int = 0) -> dict:
    key = jax.random.key(seed)
    ks = list(jax.random.split(key, 32))
    D = D_MODEL
    F = EXPERT_FF

    def nrm(i, shape, scale):
        return jax.random.normal(ks[i], shape, F32) * scale

    return {
        'x': nrm(0, (BATCH, SEQ, D), 1.0),
        'c': nrm(1, (BATCH, D), 1.0),
        'ctx': nrm(2, (BATCH, CTX_LEN, D), 1.0),
        'c_ctx': nrm(3, (D,), 1.0),
        'ada_w': nrm(4, (DEPTH, D, N_MOD * D), 0.5 * D ** -0.5),
        'ada_b': nrm(5, (DEPTH, N_MOD * D), 0.01),
        'ln_g': 1.0 + nrm(6, (DEPTH, 2, D), 0.02),
        'ln_b': nrm(7, (DEPTH, 2, D), 0.02),
        'gla_w_in': nrm(8, (N_GLA_LAYERS, D, 2 * GLA_KEY_WIDTH + 2 * GLA_VALUE_WIDTH), D ** -0.5),
        'gla_gate_w1': nrm(9, (N_GLA_LAYERS, 2, D, GLA_GATE_RANK), D ** -0.5),
        'gla_gate_w2': nrm(10, (N_GLA_LAYERS, 2, GLA_GATE_RANK, GLA_KEY_WIDTH), GLA_GATE_RANK ** -0.5),
        'gla_gate_b': nrm(11, (N_GLA_LAYERS, 2, GLA_KEY_WIDTH), 0.1),
        'gla_norm_g': 1.0 + nrm(12, (N_GLA_LAYERS, GLA_HEAD_V), 0.02),
        'gla_w_out': nrm(13, (N_GLA_LAYERS, GLA_VALUE_WIDTH, D), DEEPNORM_BETA * GLA_VALUE_WIDTH ** -0.5),
        'swa_w_qkv': nrm(14, (N_SWA_LAYERS, D, SWA_Q_WIDTH + 2 * SWA_KV_WIDTH), D ** -0.5),
        'swa_b_qkv': nrm(15, (N_SWA_LAYERS, SWA_Q_WIDTH + 2 * SWA_KV_WIDTH), 0.02),
        'swa_sink': nrm(16, (N_SWA_LAYERS, SWA_Q_HEADS), 0.5),
        'swa_w_out': nrm(17, (N_SWA_LAYERS, SWA_Q_WIDTH, D), DEEPNORM_BETA * SWA_Q_WIDTH ** -0.5),
        'swa_b_out': nrm(18, (N_SWA_LAYERS, D), 0.02),
        'conv_w_in': nrm(19, (N_CONV_LAYERS, D, 3 * D), D ** -0.5),
        'conv_w': nrm(20, (N_CONV_LAYERS, CONV_WIDTH, D), CONV_WIDTH ** -0.5),
        'conv_w_out': nrm(21, (N_CONV_LAYERS, D, D), DEEPNORM_BETA * D ** -0.5),
        'moe_router_w': nrm(22, (DEPTH, D, N_EXPERTS), D ** -0.5),
        'moe_router_b': nrm(23, (DEPTH, N_EXPERTS), 0.01),
        'moe_w_gate_up': nrm(24, (DEPTH, N_EXPERTS, D, 2 * F), D ** -0.5),
        'moe_b_gate_up': nrm(25, (DEPTH, N_EXPERTS, 2 * F), 0.02),
        'moe_w_down': nrm(26, (DEPTH, N_EXPERTS, F, D), DEEPNORM_BETA * F ** -0.5),
        'moe_b_down': nrm(27, (DEPTH, N_EXPERTS, D), 0.02),
    }


def reference(x, c, ctx, c_ctx, ada_w, ada_b, ln_g, ln_b,
              gla_w_in, gla_gate_w1, gla_gate_w2, gla_gate_b, gla_norm_g, gla_w_out,
              swa_w_qkv, swa_b_qkv, swa_sink, swa_w_out, swa_b_out,
              conv_w_in, conv_w, conv_w_out,
              moe_router_w, moe_router_b, moe_w_gate_up, moe_b_gate_up, moe_w_down, moe_b_down):
    seq = x.shape[1]
    rows = seq // GRID_W
    rope = _axial_rope_tables(rows)
    n_ctx = ctx.shape[1]
    h = jnp.concatenate([ctx, x], axis=1)
    cond_lat = jax.nn.silu(c)
    cond_ctx = jax.nn.silu(c_ctx)[None, :]

    for i in range(DEPTH):
        last = i == DEPTH - 1
        mc = [t[:, None] for t in jnp.split(cond_ctx @ ada_w[i] + ada_b[i], N_MOD, axis=-1)]
        ml = [t[:, None] for t in jnp.split(cond_lat @ ada_w[i] + ada_b[i], N_MOD, axis=-1)]

        u = _split_apply(h, n_ctx, lambda t: t * (1.0 + mc[1]) + mc[0], lambda t: t * (1.0 + ml[1]) + ml[0])
        kind, j = i % N_MIXERS, i // N_MIXERS
        if kind == 0:
            y = _gla_mixer(u, n_ctx, gla_w_in[j], gla_gate_w1[j], gla_gate_w2[j], gla_gate_b[j], gla_norm_g[j], gla_w_out[j])
        elif kind == 1:
            y = _window_mixer(u, n_ctx, swa_w_qkv[j], swa_b_qkv[j], swa_sink[j], swa_w_out[j], swa_b_out[j], rope)
        else:
            y = _conv_mixer(u, n_ctx, conv_w_in[j], conv_w[j], conv_w_out[j])
        if last:
            h, y, n_ctx = h[:, n_ctx:], y[:, n_ctx:], 0
        y = _split_apply(y, n_ctx, lambda t: t * mc[2], lambda t: t * ml[2])
        h = _layer_norm(DEEPNORM_ALPHA * h + y, ln_g[i, 0], ln_b[i, 0])

        u = _split_apply(h, n_ctx, lambda t: t * (1.0 + mc[4]) + mc[3], lambda t: t * (1.0 + ml[4]) + ml[3])
        y = _moe(u, moe_router_w[i], moe_router_b[i], moe_w_gate_up[i], moe_b_gate_up[i], moe_w_down[i], moe_b_down[i])
        y = _split_apply(y, n_ctx, lambda t: t * mc[5], lambda t: t * ml[5])
        h = _layer_norm(DEEPNORM_ALPHA * h + y, ln_g[i, 1], ln_b[i, 1])

    return h
```

```python
import numpy as np
import ml_dtypes
from contextlib import ExitStack
import concourse.bass as bass
import concourse.mybir as mybir
from concourse.bass_utils import run_bass_kernel_spmd

F32 = mybir.dt.float32
BF16 = mybir.dt.bfloat16
AF = mybir.ActivationFunctionType
ALU = mybir.AluOpType
AX = mybir.AxisListType

LN_EPS = 1e-5
SW_LIMIT = 7.0
SW_ALPHA = 1.702
MASK_NEG = -30000.0


class Cfg:
    def __init__(s, D=2048, B=4, S=4096, CTX=256, DEPTH=4, E=32, GRID_W=64):
        s.D, s.B, s.S, s.CTX, s.DEPTH, s.E, s.GRID_W = D, B, S, CTX, DEPTH, E, GRID_W
        s.KT = D // 128
        s.F = 3 * D // 8
        s.FT = s.F // 128
        s.L = CTX + S
        s.CH = CTX // 2
        s.LH = S // 2
        s.TO = s.CH + s.LH
        s.NT = s.TO // 128
        s.CT = s.CH // 128
        s.NTL = s.L // 128
        s.NCT = CTX // 128
        s.KW = D // 2
        s.DK = s.KW // 4
        s.DV = D // 4
        s.DKC = s.DK // 128
        s.DVC = s.DV // 128
        s.RANK = 16
        s.QH = D // 64
        s.KVH = s.QH // 8
        s.QHL = s.QH // 2
        s.KVL = s.KVH // 2
        s.HC = D // 2 // 128
        s.ALPHA = (2 * DEPTH) ** 0.25
        s.NG = (DEPTH + 2) // 3
        s.NS = (DEPTH + 1) // 3
        s.NC = DEPTH // 3
        s.SW_Q = s.QHL * 64
        s.SW_K = s.KVL * 128
        s.SW_KP = -(-s.SW_K // 256) * 256
        s.SW_V = s.KVL * 64
        s.SW_VP = -(-s.SW_V // 256) * 256
        s.SW_COLS = s.SW_Q + s.SW_KP + s.SW_VP
        assert s.CH % 128 == 0 and s.LH % 512 == 0 and s.DK % 128 == 0


def _merge(d, sem, val):
    k = id(sem)
    if k not in d or d[k][1] < val:
        d[k] = (sem, val)


class Tk:
    __slots__ = ("h", "w", "r", "prev", "name")

    def __init__(s, h, name=""):
        s.h = h
        s.w = {}
        s.r = {}
        s.prev = {}
        s.name = name

    def __getitem__(s, idx):
        return s.h[idx]


class DSem:
    def __init__(s, sem, step):
        s.sem = sem
        s.total = 0
        s.step = step


class Eng:
    def __init__(s, P, name, e):
        s.P = P
        s.name = name
        s.e = e
        s.sem = P.nc.semaphore("cnt_" + name).__enter__()
        s.n = 0
        s.waited = {}

    def wait_events(s, evs):
        for k, (sem, val) in evs.items():
            ds = s.P.dsem_by_id.get(k)
            if ds is not None:
                val = ds.total
            elif sem is s.sem and (s.name in ("pe", "sp")):
                continue
            if s.waited.get(k, 0) >= val:
                continue
            s.e.wait_ge(sem, val)
            s.waited[k] = val


class Prog:
    def __init__(s):
        s.nc = bass.Bass("TRN2", target_bir_lowering=False)
        nc = s.nc
        s.dsem_by_id = {}
        s.dsems = []
        s.csems = []
        s.E = {
            "pe": Eng(s, "pe", nc.tensor),
            "act": Eng(s, "act", nc.scalar),
            "dve": Eng(s, "dve", nc.vector),
            "pool": Eng(s, "pool", nc.gpsimd),
            "sp": Eng(s, "sp", nc.sync),
        }
        s.nsem = 5
        s._uid = 0
        s.relay = nc.sbuf_tensor("relay", [128, 8], F32).__enter__()

    def uid(s, p):
        s._uid += 1
        return "%s_%d" % (p, s._uid)

    def dsem(s, name, step=16, coll=False):
        sem = s.nc.semaphore(s.uid("d_" + name)).__enter__()
        ds = DSem(sem, step)
        if not coll:
            s.dsem_by_id[id(sem)] = ds
        (s.csems if coll else s.dsems).append(ds)
        s.nsem += 1
        assert s.nsem < 90, "too many semaphores"
        return ds

    def sb(s, stack, name, shape, dt):
        h = stack.enter_context(s.nc.sbuf_tensor(s.uid(name), list(shape), dt))
        return Tk(h, name)

    def ps(s, stack, name, shape, dt=F32):
        h = stack.enter_context(s.nc.psum_tensor(s.uid(name), list(shape), dt))
        return Tk(h, name)

    def dram(s, name, shape, dt, kind=None):
        if kind is None:
            h = s.nc.dram_tensor(name, list(shape), dt)
        else:
            h = s.nc.dram_tensor(name, list(shape), dt, kind=kind)
        return Tk(h, name)

    def _gather(s, R, W, WP):
        evs = {}
        for t in R:
            for sem, v in t.w.values():
                _merge(evs, sem, v)
        for t in W:
            for sem, v in t.w.values():
                _merge(evs, sem, v)
            for sem, v in t.r.values():
                _merge(evs, sem, v)
        for t in WP:
            if t.r:
                nprev = {}
                for sem, v in t.w.values():
                    _merge(nprev, sem, v)
                for sem, v in t.r.values():
                    _merge(nprev, sem, v)
                t.prev = nprev
                t.w = {}
                t.r = {}
            for sem, v in t.prev.values():
                _merge(evs, sem, v)
        return evs

    def _commit(s, ev, R, W, WP):
        for t in R:
            _merge(t.r, ev[0], ev[1])
        for t in W:
            t.w = {id(ev[0]): ev}
            t.r = {}
            t.prev = {id(ev[0]): ev}
        for t in WP:
            _merge(t.w, ev[0], ev[1])

    def op(s, en, fn, R=(), W=(), WP=()):
        E = s.E[en]
        E.wait_events(s._gather(R, W, WP))
        ins = fn(E.e)
        E.n += 1
        ins.then_inc(E.sem, 1)
        s._commit((E.sem, E.n), R, W, WP)

    def dma(s, ds, out_ap, in_ap, R=(), W=(), WP=(), q="sp", slow=False):
        Q = s.E[q]
        evs = s._gather(R, W, WP)
        if ds.total:
            evs[id(ds.sem)] = (ds.sem, ds.total)
        Q.wait_events(evs)
        if slow:
            ins = Q.e.dma_start(out=out_ap, in_=in_ap, allow_slow_non_contiguous=True)
        else:
            ins = Q.e.dma_start(out=out_ap, in_=in_ap)
        ds.total += 16
        ins.then_inc(ds.sem, 16)
        s._commit((ds.sem, ds.total), R, W, WP)

    def coll(s, cs, kind, alu, groups, ap_in, ap_out, R=(), W=()):
        Q = s.E["pool"]
        Q.wait_events(s._gather(R, W, ()))
        ins = s.nc.gpsimd.collective_compute(kind, alu, replica_groups=groups,
                                             ins=[ap_in.opt()], outs=[ap_out.opt()])
        cs.total += 1
        ins.then_inc(cs.sem, 1)
        Q.e.wait_ge(cs.sem, cs.total)
        ins2 = Q.e.memset(s.relay[:, :], 0.0)
        Q.n += 1
        ins2.then_inc(Q.sem, 1)
        s._commit((Q.sem, Q.n), R, W, ())

    def barrier(s):
        for en in ("pe", "act", "dve", "sp"):
            X = s.E[en]
            evs = {}
            for on in ("pe", "act", "dve", "sp", "pool"):
                Y = s.E[on]
                if Y is X or Y.n == 0:
                    continue
                evs[id(Y.sem)] = (Y.sem, Y.n)
            for ds in s.dsems:
                if ds.total:
                    evs[id(ds.sem)] = (ds.sem, ds.total)
            if X.n and en in ("act", "dve"):
                evs[id(X.sem)] = (X.sem, X.n)
            X.wait_events(evs)

    def final_wait(s):
        X = s.E["sp"]
        evs = {}
        for on in ("pe", "act", "dve", "pool"):
            Y = s.E[on]
            if Y.n:
                evs[id(Y.sem)] = (Y.sem, Y.n)
        for ds in s.dsems:
            if ds.total:
                evs[id(ds.sem)] = (ds.sem, ds.total)
        X.wait_events(evs)


C_IDENT, C_ONES, C_TFI, C_TFE, C_TBI, C_TBE, C_ROPEP, C_MASKB, C_SELL, C_SELC, C_HALF, NCONST = (
    0, 128, 256, 384, 512, 640, 768, 896, 1280, 1408, 1536, 1664)


def make_consts():
    a = np.zeros((128, NCONST), np.float32)
    i = np.arange(128)
    a[:, C_IDENT:C_IDENT + 128] = np.eye(128)
    a[:, C_ONES:C_ONES + 128] = 1.0
    same = (i[:, None] // 64) == (i[None, :] // 64)
    s_, c_ = i[:, None], i[None, :]
    a[:, C_TFI:C_TFI + 128] = same & (s_ <= c_)
    a[:, C_TFE:C_TFE + 128] = same & (s_ > c_)
    a[:, C_TBI:C_TBI + 128] = same & (s_ >= c_)
    a[:, C_TBE:C_TBE + 128] = same & (s_ < c_)
    pm = np.zeros((128, 128), np.float32)
    for hh in range(2):
        for ax in range(2):
            base = hh * 64 + ax * 32
            for f in range(16):
                pm[base + f, base + 16 + f] = 1.0
                pm[base + 16 + f, base + f] = 1.0
    a[:, C_ROPEP:C_ROPEP + 128] = pm
    q = i[:, None]
    j = i[None, :]
    a[:, C_MASKB:C_MASKB + 128] = np.where(j >= q, 0.0, MASK_NEG)
    a[:, C_MASKB + 128:C_MASKB + 256] = 0.0
    a[:, C_MASKB + 256:C_MASKB + 384] = np.where(j <= q, 0.0, MASK_NEG)
    a[0, C_SELL:C_SELL + 128] = 1.0
    a[1, C_SELC:C_SELC + 128] = 1.0
    a[:, C_HALF:C_HALF + 128] = 0.5
    return a


def make_rope(cfg):
    S, GW = cfg.S, cfg.GRID_W
    t = np.arange(S)
    row = (t // GW).astype(np.float32)
    col = (t % GW).astype(np.float32)
    inv = (np.float32(10000.0) ** (-np.arange(16, dtype=np.float32) / np.float32(16))).astype(np.float32)
    ang_r = (row[:, None] * inv[None, :]).astype(np.float32)
    ang_c = (col[:, None] * inv[None, :]).astype(np.float32)
    out = np.zeros((2, 128, S), np.float32)
    for hh in range(2):
        b = hh * 64
        out[0, b + 0:b + 16] = np.cos(ang_r).T
        out[0, b + 16:b + 32] = np.cos(ang_r).T
        out[0, b + 32:b + 48] = np.cos(ang_c).T
        out[0, b + 48:b + 64] = np.cos(ang_c).T
        out[1, b + 0:b + 16] = -np.sin(ang_r).T
        out[1, b + 16:b + 32] = np.sin(ang_r).T
        out[1, b + 32:b + 48] = -np.sin(ang_c).T
        out[1, b + 48:b + 64] = np.sin(ang_c).T
    return out


class Ring:
    def __init__(s, P, stack, n, name="ring"):
        s.P = P
        s.slots = [P.sb(stack, name, [128, 4096], BF16) for _ in range(n)]
        s.ds = [P.dsem(name) for _ in range(n)]
        s.i = 0
        s.n = n

    def load(s, pieces, srcs):
        k = s.i % s.n
        s.i += 1
        slot = s.slots[k]
        for dst, src in pieces:
            s.P.dma(s.ds[k], dst(slot.h), src, R=srcs, WP=[slot])
        return slot


class Pref:
    def __init__(s, ring, blocks, depth):
        s.ring, s.blocks, s.depth = ring, blocks, depth
        s.loaded = []

    def get(s, i):
        tgt = min(len(s.blocks), i + 1 + s.depth)
        while len(s.loaded) < tgt:
            pieces, srcs = s.blocks[len(s.loaded)]
            s.loaded.append(s.ring.load(pieces, srcs))
        return s.loaded[i]


class Weight:
    def __init__(s, P, name, nblk, kts, cb):
        s.name, s.nblk, s.kts, s.cb = name, nblk, kts, cb
        s.n = kts * cb
        s.trk = Tk(None, name)
        s.sh = P.dram(name + "_b", [nblk * 128, s.n], BF16)

    def sh_blk(s, j):
        return s.sh.h[j * 128:(j + 1) * 128, :]

    def blk(s, j):
        n = s.n
        return ([(lambda h, n=n: h[:, 0:n], s.sh.h[j * 128:(j + 1) * 128, :])], [s.trk])


def build(cfg, stop=None, mixers_on=(True, True, True), dbg=False, cut=None):
    c = cfg
    P = Prog()
    nc = P.nc
    D, KT, E, F, FT, DEPTH = c.D, c.KT, c.E, c.F, c.FT, c.DEPTH
    TO, NT, CT = c.TO, c.NT, c.CT
    EL = E // 2
    KS = KT // 2
    ND = D // 512

    def ext(name, shape, dt=F32):
        return P.dram(name, shape, dt, kind="ExternalInput")

    h_in = ext("h_in", [TO, D])
    cond_in = ext("cond", [2, D // 2])
    consts_in = ext("consts", [128, NCONST])
    flags_in = ext("flags", [128, 10])
    rope_in = ext("rope", [2, 128, c.S])
    ada_w_in = ext("ada_w", [DEPTH, D // 2, 6 * D])
    ada_b_in = ext("ada_b", [DEPTH, 6 * D])
    ln_g_in = ext("ln_g", [DEPTH, 2, D])
    ln_b_in = ext("ln_b", [DEPTH, 2, D])
    moe_gu_in = ext("moe_gu", [DEPTH, EL, D, 2 * F])
    moe_d_in = ext("moe_d", [DEPTH, EL, F, D])
    moe_rw_in = ext("moe_rw", [DEPTH, D, E])
    moe_rb_in = ext("moe_rb", [DEPTH, E])
    moe_bgu_in = ext("moe_bgu", [DEPTH, EL, 2 * F])
    moe_bd_in = ext("moe_bd", [DEPTH, EL, D])
    GLW = 4 * c.DK + 4 * c.DV
    gla_win_in = ext("gla_win", [c.NG, D, GLW])
    gla_w1_in = ext("gla_w1", [c.NG, 2, D, c.RANK])
    gla_w2_in = ext("gla_w2", [c.NG, 2, c.RANK, 2 * c.DK])
    gla_gb_in = ext("gla_gb", [c.NG, 2, 2 * c.DK])
    gla_ng_in = ext("gla_ng", [c.NG, c.DV])
    gla_wout_in = ext("gla_wout", [c.NG, 2 * c.DV, D])
    swa_win_in = ext("swa_win", [c.NS, D, c.SW_COLS])
    swa_bq_in = ext("swa_bq", [c.NS, c.SW_Q])
    swa_bk_in = ext("swa_bk", [c.NS, c.SW_K])
    swa_bv_in = ext("swa_bv", [c.NS, 128])
    swa_sink_in = ext("swa_sink", [c.NS, c.QHL])
    swa_wout_in = ext("swa_wout", [c.NS, D // 2, D])
    swa_bout_in = ext("swa_bout", [c.NS, D])
    conv_win_in = ext("conv_win", [max(c.NC, 1), D, 3 * D // 2])
    conv_w_in = ext("conv_w", [max(c.NC, 1), 3, D // 2])
    conv_wout_in = ext("conv_wout", [max(c.NC, 1), D // 2, D])
    out_t = P.dram("out", [TO, D], F32, kind="ExternalOutput")

    def finish():
        P.final_wait()
        return nc

    h_tiles = [P.dram("h_%d" % t, [128, D], F32) for t in range(NT)]
    y_own = P.dram("y_own", [TO, D], F32)
    yp = P.dram("yp", [2 * TO, D], F32)
    uT_pair_d = [P.dram("uT_pair%d" % m, [NT * 128, KT * 128], BF16) for m in range(2)]
    xbuf_d = [P.dram("xbuf%d" % m, [2 * NT * 128, KT * 128], BF16) for m in range(2)]
    uT_pair = Tk(None, "uT_pair")
    xbuf = Tk(None, "xbuf")
    NMR = DEPTH * 6 * 2
    modbuf = P.dram("modbuf", [2 * NMR, D], F32)
    modrows = P.dram("modrows", [NMR, D], F32)
    gxbuf_d = [P.dram("gxbuf%d" % m, [2 * NT * 128, E], F32) for m in range(2)]
    gpair_d = [P.dram("gpair%d" % m, [NT * 128, E], F32) for m in range(2)]
    gxbuf = Tk(None, "gxbuf")
    gpair = Tk(None, "gpair")
    ofwd = P.dram("ofwd", [c.NTL * 128, 2 * c.DVC * 128], F32)

    GALL = [list(range(8))]
    GPAIR = [[0, 1], [2, 3], [4, 5], [6, 7]]

    pst = ExitStack()
    cst = P.sb(pst, "consts", [128, NCONST], F32)
    ident_bf = P.sb(pst, "ident_bf", [128, 128], BF16)
    ones_bf = P.sb(pst, "ones_bf", [128, 128], BF16)
    mask_f_bf = P.sb(pst, "mask_f", [128, 128], BF16)
    mask_b_bf = P.sb(pst, "mask_b", [128, 128], BF16)
    G_all = P.sb(pst, "G_all", [128, NT, E], F32)
    flg = P.sb(pst, "flg", [128, 10], F32)
    ds_misc = P.dsem("misc")
    ds_st = [P.dsem("st0"), P.dsem("st1")]
    ds_ld = [P.dsem("ld0"), P.dsem("ld1"), P.dsem("ld2"), P.dsem("ld3")]
    cs_a = P.dsem("ca", step=1, coll=True)

    def CS(off, n=128, p0=0, p1=128):
        return cst[p0:p1, off:off + n]

    P.dma(ds_misc, cst[:, :], consts_in[:, :], R=[consts_in], W=[cst])
    P.dma(ds_misc, flg[:, :], flags_in[:, :], R=[flags_in], W=[flg])
    P.op("dve", lambda e: e.tensor_copy(ident_bf[:, :], CS(C_IDENT)), R=[cst], W=[ident_bf])
    P.op("dve", lambda e: e.tensor_copy(ones_bf[:, :], CS(C_ONES)), R=[cst], W=[ones_bf])
    P.op("dve", lambda e: e.tensor_copy(mask_f_bf[:, :], CS(C_TFI)), R=[cst], W=[mask_f_bf])
    P.op("dve", lambda e: e.tensor_copy(mask_b_bf[:, :], CS(C_TBI)), R=[cst], W=[mask_b_bf])

    W_ada = [Weight(P, "ada%d" % i, 6 * D // 256, KS, 256) for i in range(DEPTH)]
    W_gu = [Weight(P, "gu%d" % i, EL * FT, KT, 256) for i in range(DEPTH)]
    W_dn = [Weight(P, "dn%d" % i, EL * ND, FT, 512) for i in range(DEPTH)]
    W_glin = [Weight(P, "glin%d" % j, GLW // 256, KT, 256) for j in range(c.NG)]
    W_glout = [Weight(P, "glout%d" % j, ND, 2 * c.DVC, 512) for j in range(c.NG)]
    W_swin = [Weight(P, "swin%d" % j, c.SW_COLS // 256, KT, 256) for j in range(c.NS)]
    W_swout = [Weight(P, "swout%d" % j, ND, c.HC, 512) for j in range(c.NS)]
    W_cvin = [Weight(P, "cvin%d" % j, (3 * D // 2) // 256, KT, 256) for j in range(c.NC)]
    W_cvout = [Weight(P, "cvout%d" % j, ND, c.HC, 512) for j in range(c.NC)]

    def rows_src(tk, lead, cb):
        def f(j):
            src = tk.h[lead][:, j * cb:(j + 1) * cb].rearrange("(ks p) c -> p ks c", p=128)
            return [(0, cb, src)]
        return f

    def prep(stack_bufs, w, srcfn, src_tk, eng_i):
        st32, st16, dsl, dss = stack_bufs
        for j in range(w.nblk):
            k = eng_i[0] % 2
            eng_i[0] += 1
            v32 = st32[k][:, 0:w.n].rearrange("p (ks c) -> p ks c", c=w.cb)
            for (c0, c1, src) in srcfn(j):
                P.dma(dsl[k], v32[:, :, c0:c1], src, R=[src_tk], WP=[st32[k]])
            if eng_i[0] % 2:
                P.op("act", lambda e: e.copy(st16[k][:, 0:w.n], st32[k][:, 0:w.n]), R=[st32[k]], W=[st16[k]])
            else:
                P.op("dve", lambda e: e.tensor_copy(st16[k][:, 0:w.n], st32[k][:, 0:w.n]), R=[st32[k]], W=[st16[k]])
            P.dma(dss[k], w.sh_blk(j), st16[k][:, 0:w.n], R=[st16[k]], WP=[w.trk], q="act")

    def mixer_kind(i):
        import os
        if os.environ.get("MIXK"):
            return int(os.environ["MIXK"]), 0
        return i % 3, i // 3

    with ExitStack() as st:
        st32 = [P.sb(st, "st32", [128, 4096], F32) for _ in range(2)]
        st16 = [P.sb(st, "st16", [128, 4096], BF16) for _ in range(2)]
        bufs = (st32, st16, ds_ld[0:2], ds_st)
        cnt = [0]
        for i in range(DEPTH):
            prep(bufs, W_ada[i], rows_src(ada_w_in, i, 256), ada_w_in, cnt)
        for i in range(DEPTH):
            kind, j = mixer_kind(i)
            if kind == 0 and mixers_on[0]:
                prep(bufs, W_glin[j], rows_src(gla_win_in, j, 256), gla_win_in, cnt)
                prep(bufs, W_glout[j], rows_src(gla_wout_in, j, 512), gla_wout_in, cnt)
            elif kind == 1 and mixers_on[1]:
                prep(bufs, W_swin[j], rows_src(swa_win_in, j, 256), swa_win_in, cnt)
                prep(bufs, W_swout[j], rows_src(swa_wout_in, j, 512), swa_wout_in, cnt)
            elif kind == 2 and mixers_on[2]:
                prep(bufs, W_cvin[j], rows_src(conv_win_in, j, 256), conv_win_in, cnt)
                prep(bufs, W_cvout[j], rows_src(conv_wout_in, j, 512), conv_wout_in, cnt)

            def gu_src(jb, i=i):
                e, b = jb // FT, jb % FT
                m = moe_gu_in.h[i, e]
                return [(0, 128, m[:, b * 128:(b + 1) * 128].rearrange("(ks p) c -> p ks c", p=128)),
                        (128, 256, m[:, F + b * 128:F + (b + 1) * 128].rearrange("(ks p) c -> p ks c", p=128))]

            def dn_src(jb, i=i):
                e, b = jb // ND, jb % ND
                m = moe_d_in.h[i, e]
                return [(0, 512, m[:, b * 512:(b + 1) * 512].rearrange("(ks p) c -> p ks c", p=128))]

            prep(bufs, W_gu[i], gu_src, moe_gu_in, cnt)
            prep(bufs, W_dn[i], dn_src, moe_d_in, cnt)
        P.barrier()
    if cut == "prep":
        return finish()

    with ExitStack() as st:
        ring = Ring(P, st, 4, "ringm")
        DS2 = D // 2
        cnd = P.sb(st, "cnd", [2, DS2], F32)
        cnds = P.sb(st, "cnds", [2, DS2], F32)
        condT = P.sb(st, "condT", [128, KS, 2], BF16)
        rows = [P.sb(st, "rows", [2, D], F32) for _ in range(2)]
        pT = P.ps(st, "pT", [128, KS, 2], F32)
        pr = [P.ps(st, "pr", [2, 512], F32) for _ in range(4)]
        P.dma(ds_misc, cnd[:, :], cond_in[:, :], R=[cond_in], W=[cnd])
        P.op("act", lambda e: e.activation(out=cnds[:, :], in_=cnd[:, :], func=AF.Silu), R=[cnd], W=[cnds])
        for k in range(KS):
            P.op("pe", lambda e, k=k: e.transpose(pT[:, k, :], cnds[0:2, k * 128:(k + 1) * 128], CS(C_IDENT, 2, 0, 2)),
                 R=[cnds, cst], WP=[pT])
        P.op("dve", lambda e: e.tensor_copy(condT[:, :, :], pT[:, :, :]), R=[pT], W=[condT])
        nb = D // 256
        nrow = 0
        for i in range(DEPTH):
            blocks = [W_ada[i].blk(j) for j in range(6 * nb)]
            pf = Pref(ring, blocks, 3)
            for m in range(6):
                rw = rows[nrow % 2]
                nrow += 1
                for jb in range(nb):
                    slot = pf.get(m * nb + jb)
                    sv = slot.h[:, 0:KS * 256].rearrange("p (k c) -> p k c", c=256)
                    pt = pr[(jb // 2) % 4]
                    o = pt[:, (jb % 2) * 256:(jb % 2) * 256 + 256]
                    for k in range(KS):
                        P.op("pe", lambda e, k=k, o=o, sv=sv: e.matmul(o, condT[:, k, :], sv[:, k, :], start=(k == 0), stop=(k == KS - 1)),
                             R=[condT, slot], WP=[pt])
                    if jb % 2 == 1:
                        q = jb // 2
                        P.op("act", lambda e, q=q, pt=pt, rw=rw: e.copy(rw[:, q * 512:(q + 1) * 512], pt[:, :]), R=[pt], WP=[rw])
                for hf in range(2):
                    base = hf * NMR + (i * 6 + m) * 2
                    P.dma(ds_st[hf], modbuf.h[base:base + 2, :], rw[0:2, :], R=[rw], WP=[modbuf], q="act")
        P.coll(cs_a, "ReduceScatter", ALU.add, GPAIR, modbuf.h.ap(), modrows.h.ap(), R=[modbuf], W=[modrows])
        P.barrier()
    if cut == "mods":
        return finish()

    def load_modrow(dst, i, m):
        base = (i * 6 + m) * 2
        P.dma(ds_misc, dst[0:2, :], modrows.h[base:base + 2, :], R=[modrows], W=[dst])

    def add_adab(dst, tmp, i, m):
        for rr in range(2):
            P.dma(ds_misc, tmp[rr:rr + 1, :], ada_b_in[i:i + 1, m * D:(m + 1) * D], R=[ada_b_in], WP=[tmp])
        P.op("dve", lambda e: e.tensor_tensor(dst[0:2, :], dst[0:2, :], tmp[0:2, :], ALU.add), R=[tmp], W=[dst])

    def seq_tile(q):
        if q < c.NCT:
            return q // CT, q % CT
        q2 = q - c.NCT
        nl = c.LH // 128
        return q2 // nl, CT + q2 % nl

    def token_stage(i, s_idx, first=False):
        final = (not first) and (i == DEPTH - 1) and (s_idx == 1)
        if first:
            nl, nm, to_moe = 0, 0, False
        elif s_idx == 0:
            nl, nm, to_moe = i, 3, True
        else:
            nl, nm, to_moe = i + 1, 0, False
        with ExitStack() as st:
            r_sh = P.sb(st, "r_sh", [2, D], F32)
            r_sc = P.sb(st, "r_sc", [2, D], F32)
            shc = P.sb(st, "shc", [128, KT, 2], F32)
            scc = P.sb(st, "scc", [128, KT, 2], F32)
            pcol = P.ps(st, "pcol", [128, KT, 2], F32)
            pbc = [P.ps(st, "pbc", [128, 512], F32) for _ in range(2)]
            ptr = [P.ps(st, "ptr", [128, 512], F32) for _ in range(4)]
            import os
            r_tmp = P.sb(st, "r_tmp", [2, D], F32)
            if not final:
                load_modrow(r_sh, nl, nm)
                add_adab(r_sh, r_tmp, nl, nm)
                load_modrow(r_sc, nl, nm + 1)
                add_adab(r_sc, r_tmp, nl, nm + 1)
                for rr, cc, add1 in ((r_sh, shc, False), (r_sc, scc, True)):
                    for k in range(KT):
                        P.op("pe", lambda e, k=k, rr=rr: e.matmul(pcol[:, k, :], rr[0:2, k * 128:(k + 1) * 128], CS(C_IDENT, 2, 0, 2), start=True, stop=True),
                             R=[rr, cst], WP=[pcol])
                    if add1:
                        P.op("dve", lambda e, cc=cc: e.tensor_scalar_add(cc[:, :, :], pcol[:, :, :], 1.0), R=[pcol], W=[cc])
                    else:
                        P.op("dve", lambda e, cc=cc: e.tensor_copy(cc[:, :, :], pcol[:, :, :]), R=[pcol], W=[cc])
            if not first:
                r_g = P.sb(st, "r_g", [2, D], F32)
                r_ln = P.sb(st, "r_ln", [1, 2, D], F32)
                gbc = [P.sb(st, "gbc", [128, D], F32) for _ in range(2)]
                lng = P.sb(st, "lng", [128, D], F32)
                lnb = P.sb(st, "lnb", [128, D], F32)
                load_modrow(r_g, i, 2 + 3 * s_idx)
                add_adab(r_g, r_tmp, i, 2 + 3 * s_idx)
                P.dma(ds_misc, r_ln[0:1, 0, :], ln_g_in[i, s_idx:s_idx + 1, :], R=[ln_g_in], WP=[r_ln])
                P.dma(ds_misc, r_ln[0:1, 1, :], ln_b_in[i, s_idx:s_idx + 1, :], R=[ln_b_in], WP=[r_ln])
                n = 0
                for dst, lhs, rhs_fn, rt in () if "b" in os.environ.get("TS_SKIP", "") else (
                        (gbc[0], CS(C_SELL, 128, 0, 2), lambda a, b: r_g[0:2, a:b], r_g),
                        (gbc[1], CS(C_SELC, 128, 0, 2), lambda a, b: r_g[0:2, a:b], r_g),
                        (lng, CS(C_ONES, 128, 0, 1), lambda a, b: r_ln[0:1, 0, a:b], r_ln),
                        (lnb, CS(C_ONES, 128, 0, 1), lambda a, b: r_ln[0:1, 1, a:b], r_ln)):
                    for db in range(D // 512):
                        pb = pbc[n % 2]
                        n += 1
                        P.op("pe", lambda e, pb=pb, lhs=lhs, rhs_fn=rhs_fn, db=db: e.matmul(pb[:, :], lhs, rhs_fn(db * 512, (db + 1) * 512), start=True, stop=True),
                             R=[cst, rt], W=[pb])
                        P.op("act", lambda e, pb=pb, dst=dst, db=db: e.copy(dst[:, db * 512:(db + 1) * 512], pb[:, :]), R=[pb], WP=[dst])
            if to_moe:
                wr = P.sb(st, "wr", [128, KT, E], F32)
                rb = P.sb(st, "rb", [1, E], F32)
                plg = P.ps(st, "plg", [128, E], F32)
                if "r" not in os.environ.get("TS_SKIP", ""):
                    P.dma(ds_misc, wr[:, :, :], moe_rw_in.h[i].rearrange("(k p) e -> p k e", p=128), R=[moe_rw_in], W=[wr])
                    P.dma(ds_misc, rb[:, :], moe_rb_in[i:i + 1, :], R=[moe_rb_in], W=[rb])
            NB = 2
            yt = [P.sb(st, "yt", [128, D], F32) for _ in range(NB)]
            ht = [P.sb(st, "ht", [128, D], F32) for _ in range(NB)]
            zt = [P.sb(st, "zt", [128, D], F32) for _ in range(NB)]
            uTt = [P.sb(st, "uTt", [128, KT, 128], BF16) for _ in range(NB)]
            um = [P.sb(st, "um", [128, KT * 128], BF16) for _ in range(2)]
            gm = P.sb(st, "gm", [128, 2, E], F32) if to_moe else None
            u32 = [P.sb(st, "u32", [128, KT, 128], F32) for _ in range(NB)] if to_moe else None
            stats = P.sb(st, "stats", [128, D // 512, 6], F32)
            mv = P.sb(st, "mv", [128, 2], F32)
            sm = P.sb(st, "sm", [128, 8], F32)
            lg = P.sb(st, "lg", [128, E], F32)
            lg2 = P.sb(st, "lg2", [128, E], F32)
            top8 = P.sb(st, "top8", [128, 8], F32)
            import os
            TSC = int(os.environ.get("TS_CUT", "9")) if not first else 9
            for t in range(NT if TSC > 0 else 0):
                b = t % NB
                kind = 1 if t < CT else 0
                hT_, yT_, zT_ = ht[b], yt[b], zt[b]
                P.dma(ds_ld[b], hT_[:, :], (h_in[t * 128:(t + 1) * 128, :] if first else h_tiles[t][:, :]),
                      R=[h_in if first else h_tiles[t]], W=[hT_])
                if not first:
                    P.dma(ds_ld[2 + b], yT_[:, :], y_own[t * 128:(t + 1) * 128, :], R=[y_own], W=[yT_])
                    P.op("dve", lambda e: e.tensor_tensor(yT_[:, :], yT_[:, :], gbc[kind][:, :], ALU.mult), R=[gbc[kind]], W=[yT_])
                    P.op("dve", lambda e: e.scalar_tensor_tensor(zT_[:, :], hT_[:, :], float(c.ALPHA), yT_[:, :], ALU.mult, ALU.add),
                         R=[hT_, yT_], W=[zT_])
                    for q in range(D // 512):
                        P.op("dve", lambda e, q=q: e.bn_stats(stats[:, q, :], zT_[:, q * 512:(q + 1) * 512]), R=[zT_], WP=[stats])
                    P.op("dve", lambda e: e.bn_aggr(mv[:, :], stats[:, :, :]), R=[stats], W=[mv])
                    P.op("dve", lambda e: e.tensor_scalar_add(sm[:, 0:1], mv[:, 1:2], LN_EPS), R=[mv], WP=[sm])
                    P.op("act", lambda e: e.sqrt(sm[:, 1:2], sm[:, 0:1]), R=[sm], WP=[sm])
                    P.op("dve", lambda e: e.reciprocal(sm[:, 2:3], sm[:, 1:2]), R=[sm], WP=[sm])
                    P.op("dve", lambda e: e.tensor_scalar(sm[:, 3:4], mv[:, 0:1], sm[:, 2:3], -1.0, ALU.mult, ALU.mult), R=[sm, mv], WP=[sm])
                    P.op("act", lambda e: e.activation(out=yT_[:, :], in_=zT_[:, :], func=AF.Identity, bias=sm[:, 3:4], scale=sm[:, 2:3]),
                         R=[zT_, sm], W=[yT_])
                    P.op("dve", lambda e: e.tensor_tensor(yT_[:, :], yT_[:, :], lng[:, :], ALU.mult), R=[lng], W=[yT_])
                    P.op("dve", lambda e: e.tensor_tensor(hT_[:, :], yT_[:, :], lnb[:, :], ALU.add), R=[yT_, lnb], W=[hT_])
                    if final:
                        P.dma(ds_st[b], out_t[t * 128:(t + 1) * 128, :], hT_[:, :], R=[hT_], WP=[out_t], q="act")
                        continue
                    P.dma(ds_st[b], h_tiles[t][:, :], hT_[:, :], R=[hT_], W=[h_tiles[t]], q="act")
                    if TSC <= 1:
                        continue
                else:
                    P.dma(ds_st[b], h_tiles[t][:, :], hT_[:, :], R=[hT_], W=[h_tiles[t]], q="act")
                for k in range(KT):
                    pq = ptr[k // 4]
                    P.op("pe", lambda e, k=k, pq=pq: e.transpose(pq[:, (k % 4) * 128:(k % 4) * 128 + 128], hT_[:, k * 128:(k + 1) * 128], CS(C_IDENT)),
                         R=[hT_, cst], WP=[pq])
                for k in range(KT):
                    pq = ptr[k // 4]
                    src = pq[:, (k % 4) * 128:(k % 4) * 128 + 128]
                    P.op("act", lambda e, k=k, src=src: e.activation(out=uTt[b][:, k, :], in_=src, func=AF.Identity,
                                                                     bias=shc[:, k, kind:kind + 1], scale=scc[:, k, kind:kind + 1]),
                         R=[pq, shc, scc], WP=[uTt[b]])
                    if to_moe:
                        P.op("act", lambda e, k=k, src=src: e.activation(out=u32[b][:, k, :], in_=src, func=AF.Identity,
                                                                         bias=shc[:, k, kind:kind + 1], scale=scc[:, k, kind:kind + 1]),
                             R=[pq, shc, scc], WP=[u32[b]])
                uflat = uTt[b][:, :, :].rearrange("p k c -> p (k c)")
                if TSC <= 2:
                    continue
                if True:
                    for m_ in range(2):
                        P.op("dve", lambda e, m_=m_: e.tensor_scalar_mul(um[m_][:, :], uflat, flg[:, m_:m_ + 1]),
                             R=[uTt[b], flg], W=[um[m_]])
                        for hf in range(2):
                            r0 = (hf * NT + t) * 128
                            P.dma(ds_st[b], xbuf_d[m_].h[r0:r0 + 128, :], um[m_][:, :], R=[um[m_]], WP=[xbuf], q="act")
                if TSC <= 3:
                    continue
                if to_moe:
                    for k in range(KT):
                        P.op("pe", lambda e, k=k: e.matmul(plg[:, :], u32[b][:, k, :], wr[:, k, :], start=(k == 0), stop=False), R=[u32[b], wr], WP=[plg])
                    P.op("pe", lambda e: e.matmul(plg[:, :], CS(C_ONES, 128, 0, 1), rb[0:1, :], start=False, stop=True), R=[cst, rb], WP=[plg])
                    P.op("act", lambda e: e.copy(lg[:, :], plg[:, :]), R=[plg], W=[lg])
                    P.op("dve", lambda e: e.max(out=top8[:, :], in_=lg[:, :]), R=[lg], W=[top8])
                    P.op("dve", lambda e: e.tensor_scalar_mul(sm[:, 4:5], top8[:, 0:1], -1.0), R=[top8], WP=[sm])
                    P.op("act", lambda e: e.activation(out=lg2[:, :], in_=lg[:, :], func=AF.Exp, bias=sm[:, 4:5], scale=1.0), R=[lg, sm], W=[lg2])
                    P.op("dve", lambda e: e.tensor_scalar(lg[:, :], lg[:, :], top8[:, 3:4], None, ALU.is_ge), R=[top8], W=[lg])
                    P.op("dve", lambda e: e.tensor_tensor(lg2[:, :], lg2[:, :], lg[:, :], ALU.mult), R=[lg], W=[lg2])
                    P.op("dve", lambda e: e.reduce_sum(sm[:, 5:6], lg2[:, :], axis=AX.X), R=[lg2], WP=[sm])
                    P.op("dve", lambda e: e.reciprocal(sm[:, 6:7], sm[:, 5:6]), R=[sm], WP=[sm])
                    P.op("dve", lambda e: e.tensor_scalar_mul(G_all[:, t, :], lg2[:, :], sm[:, 6:7]), R=[lg2, sm], WP=[G_all])
                    for m_ in range(2):
                        P.op("dve", lambda e, m_=m_: e.tensor_scalar_mul(gm[:, m_, :], G_all[:, t, :], flg[:, m_:m_ + 1]),
                             R=[G_all, flg], WP=[gm])
                    for m_ in range(2):
                        for hf in range(2):
                            r0 = (hf * NT + t) * 128
                            P.dma(ds_st[b], gxbuf_d[m_].h[r0:r0 + 128, :], gm[:, m_, :], R=[gm], WP=[gxbuf], q="act")
            P.barrier()


    def moe_stage(i):
        alltiles = [(m, lt) for m in range(2) for lt in range(NT)]
        groups = [alltiles[a:a + 4] for a in range(0, len(alltiles), 4)]
        with ExitStack() as st:
            ring = Ring(P, st, 10, "ringe")
            bgr = P.sb(st, "bgr", [EL, 2 * F], F32)
            bguT = P.sb(st, "bguT", [128, 2 * FT, EL], F32)
            bd = P.sb(st, "bd", [EL, D], F32)
            GX = P.sb(st, "GX", [128, 2 * NT, E], F32)
            GL = P.sb(st, "GL", [128, 2 * NT, EL], F32)
            uTg = [P.sb(st, "uTg", [128, KT, 512], BF16) for _ in range(2)]
            acc = P.sb(st, "acc", [128, 4, D], F32)
            acck = [[Tk(acc.h, "acc") for _ in range(ND)] for _ in range(4)]
            GT = P.sb(st, "GT", [EL, 4, 128], F32)
            actT = [P.sb(st, "actT", [128, FT, 512], BF16) for _ in range(2)]
            gl = [P.sb(st, "gl", [128, 512], F32) for _ in range(2)]
            sg = [P.sb(st, "sg", [128, 512], F32) for _ in range(2)]
            l1 = [P.sb(st, "l1", [128, 512], F32) for _ in range(2)]
            pg = [P.ps(st, "pg", [128, 512], F32) for _ in range(2)]
            pl = [P.ps(st, "pl", [128, 512], F32) for _ in range(2)]
            pd = [P.ps(st, "pd", [128, 512], F32) for _ in range(2)]
            pm = P.ps(st, "pm", [128, 512], F32)
            P.dma(ds_misc, bgr[:, :], moe_bgu_in.h[i], R=[moe_bgu_in], W=[bgr])
            P.dma(ds_misc, bd[:, :], moe_bd_in.h[i], R=[moe_bd_in], W=[bd])
            for m in range(2):
                P.dma(ds_misc, GX[:, m * NT:(m + 1) * NT, :], gpair_d[m].h[:, :].rearrange("(tt p) e -> p tt e", p=128), R=[gpair], WP=[GX])
            P.op("dve", lambda e: e.tensor_scalar_mul(GL[:, :, :], GX[:, :, 0:EL], flg[:, 0:1]), R=[GX, flg], W=[GL])
            P.op("dve", lambda e: e.scalar_tensor_tensor(GL[:, :, :], GX[:, :, EL:2 * EL], flg[:, 1:2], GL[:, :, :], ALU.mult, ALU.add),
                 R=[GX, flg], W=[GL])
            for q in range(2 * FT):
                P.op("pe", lambda e, q=q: e.transpose(pm[:, 0:EL], bgr[:, q * 128:(q + 1) * 128], CS(C_IDENT, EL, 0, EL)), R=[bgr, cst], W=[pm])
                P.op("act", lambda e, q=q: e.copy(bguT[:, q, :], pm[:, 0:EL]), R=[pm], WP=[bguT])
            blocks = []
            for g_ in groups:
                for ex in range(EL):
                    for b_ in range(FT):
                        blocks.append(W_gu[i].blk(ex * FT + b_))
                    for b_ in range(ND):
                        blocks.append(W_dn[i].blk(ex * ND + b_))
            pf = Pref(ring, blocks, 8)
            bi = 0
            cnt = 0
            for gi, g_ in enumerate(groups):
                ng = len(g_)
                N = ng * 128
                ug = uTg[gi % 2]
                for a, (m, lt) in enumerate(g_):
                    P.dma(ds_ld[gi % 2], ug[:, :, a * 128:(a + 1) * 128],
                          uT_pair_d[m].h[lt * 128:(lt + 1) * 128, :].rearrange("p (k c) -> p k c", c=128), R=[uT_pair], WP=[ug])
                for a, (m, lt) in enumerate(g_):
                    gt = m * NT + lt
                    P.op("pe", lambda e, gt=gt: e.transpose(pm[0:EL, 0:128], GL[:, gt, :], CS(C_IDENT)), R=[GL, cst], W=[pm])
                    P.op("act", lambda e, a=a: e.copy(GT[:, a, :], pm[0:EL, 0:128]), R=[pm], WP=[GT])
                for a in range(ng):
                    for db in range(ND):
                        pp = pd[cnt % 2]
                        cnt += 1
                        P.op("pe", lambda e, pp=pp, a=a, db=db: e.matmul(pp[:, :], GT[:, a, :], bd[:, db * 512:(db + 1) * 512], start=True, stop=True),
                             R=[GT, bd], W=[pp])
                        P.op("act", lambda e, pp=pp, a=a, db=db: e.copy(acc[:, a, db * 512:(db + 1) * 512], pp[:, :]), R=[pp], W=[acck[a][db]])
                for ex in range(EL):
                    aT = actT[ex % 2]
                    for b_ in range(FT):
                        slot = pf.get(bi)
                        bi += 1
                        sv = slot.h[:, :].rearrange("p (k c) -> p k c", c=256)
                        k2 = (ex * FT + b_) % 2
                        pG, pL, gL, sG, lL = pg[k2], pl[k2], gl[k2], sg[k2], l1[k2]
                        for k in range(KT):
                            P.op("pe", lambda e, k=k: e.matmul(pG[:, 0:N], sv[:, k, 0:128], ug[:, k, 0:N], start=(k == 0), stop=(k == KT - 1)),
                                 R=[slot, ug], WP=[pG])
                        for k in range(KT):
                            P.op("pe", lambda e, k=k: e.matmul(pL[:, 0:N], sv[:, k, 128:256], ug[:, k, 0:N], start=(k == 0), stop=(k == KT - 1)),
                                 R=[slot, ug], WP=[pL])
                        P.op("dve", lambda e: e.tensor_scalar(gL[:, 0:N], pG[:, 0:N], bguT[:, b_, ex:ex + 1], SW_LIMIT, ALU.add, ALU.min),
                             R=[pG, bguT], W=[gL])
                        P.op("act", lambda e: e.activation(out=sG[:, 0:N], in_=gL[:, 0:N], func=AF.Sigmoid, scale=SW_ALPHA), R=[gL], W=[sG])
                        P.op("dve", lambda e: e.tensor_scalar(lL[:, 0:N], pL[:, 0:N], bguT[:, FT + b_, ex:ex + 1], SW_LIMIT, ALU.add, ALU.min),
                             R=[pL, bguT], W=[lL])
                        P.op("dve", lambda e: e.tensor_scalar(lL[:, 0:N], lL[:, 0:N], -SW_LIMIT, 1.0, ALU.max, ALU.add), W=[lL])
                        P.op("dve", lambda e: e.tensor_tensor(gL[:, 0:N], gL[:, 0:N], sG[:, 0:N], ALU.mult), R=[sG], W=[gL])
                        P.op("dve", lambda e: e.tensor_tensor(aT[:, b_, 0:N], gL[:, 0:N], lL[:, 0:N], ALU.mult), R=[gL, lL], WP=[aT])
                    for db in range(ND):
                        slot = pf.get(bi)
                        bi += 1
                        sv = slot.h[:, 0:FT * 512].rearrange("p (k c) -> p k c", c=512)
                        for a, (m, lt) in enumerate(g_):
                            gt = m * NT + lt
                            pp = pd[cnt % 2]
                            cnt += 1
                            for f_ in range(FT):
                                P.op("pe", lambda e, f_=f_: e.matmul(pp[:, :], aT[:, f_, a * 128:(a + 1) * 128], sv[:, f_, :], start=(f_ == 0), stop=(f_ == FT - 1)),
                                     R=[aT, slot], WP=[pp])
                            P.op("dve", lambda e: e.scalar_tensor_tensor(acc[:, a, db * 512:(db + 1) * 512], pp[:, :], GL[:, gt, ex:ex + 1],
                                                                         acc[:, a, db * 512:(db + 1) * 512], ALU.mult, ALU.add),
                                 R=[pp, GL], W=[acck[a][db]])
                for a, (m, lt) in enumerate(g_):
                    r0 = (m * NT + lt) * 128
                    P.dma(ds_st[a % 2], yp.h[r0:r0 + 128, :], acc[:, a, :], R=acck[a], WP=[yp], q="act")
            P.barrier()


    def seq_groups():
        gs = []
        for (a0, b0) in ((0, c.NCT), (c.NCT, c.NTL)):
            q = a0
            while q < b0:
                n = min(4, b0 - q)
                gs.append((q, n))
                q += n
        return gs

    def ucols_of(q):
        rk, lt = seq_tile(q)
        return uT_pair_d[rk].h[lt * 128:(lt + 1) * 128, :].rearrange("p (k c) -> p k c", c=128)

    def swa_stage(i, j):
        QHL, KVL, HC, L, CTX, S = c.QHL, c.KVL, c.HC, c.L, c.CTX, c.S
        NHP = QHL // 2
        VW = KVL * 64
        NLT = S // 128
        groups = seq_groups()
        with ExitStack() as st:
            ring = Ring(P, st, 6, "rings")
            rc = P.sb(st, "rc", [128, S], F32)
            rsn = P.sb(st, "rsn", [128, S], F32)
            P.dma(ds_misc, rc[:, :], rope_in.h[0], R=[rope_in], W=[rc])
            P.dma(ds_misc, rsn[:, :], rope_in.h[1], R=[rope_in], W=[rsn])
            bq = P.sb(st, "bq", [128, NHP], F32)
            bk = P.sb(st, "bk", [128, KVL], F32)
            bvr = P.sb(st, "bvr", [1, 128], F32)
            bvb = P.sb(st, "bvb", [1, 128], BF16)
            skr = P.sb(st, "skr", [1, QHL], F32)
            skb = P.sb(st, "skb", [128, QHL], F32)
            bor = P.sb(st, "bor", [1, D], F32)
            P.dma(ds_misc, bq[:, :], swa_bq_in.h[j].rearrange("(hp p) -> p hp", p=128), R=[swa_bq_in], W=[bq], slow=True)
            P.dma(ds_misc, bk[:, :], swa_bk_in.h[j].rearrange("(hp p) -> p hp", p=128), R=[swa_bk_in], W=[bk], slow=True)
            P.dma(ds_misc, bvr[:, :], swa_bv_in[j:j + 1, :], R=[swa_bv_in], W=[bvr])
            P.dma(ds_misc, skr[:, :], swa_sink_in[j:j + 1, :], R=[swa_sink_in], W=[skr])
            P.dma(ds_misc, bor[:, :], swa_bout_in[j:j + 1, :], R=[swa_bout_in], W=[bor])
            P.op("dve", lambda e: e.tensor_copy(bvb[:, :], bvr[:, :]), R=[bvr], W=[bvb])
            KTr = [P.sb(st, "KTr", [128, L], BF16) for _ in range(KVL)]
            Vr = P.sb(st, "Vr", [128, c.NTL, VW], BF16)
            U = [P.sb(st, "U", [128, KT, 512], BF16) for _ in range(2)]
            xf = [P.sb(st, "xf", [128, 512], F32) for _ in range(2)]
            t1 = [P.sb(st, "t1", [128, 512], F32) for _ in range(2)]
            QT = P.sb(st, "QT", [128, NHP, 512], BF16)
            sc = [P.sb(st, "sc", [128, 640], F32) for _ in range(2)]
            pb = [P.sb(st, "pb", [128, 640], BF16) for _ in range(2)]
            pTs = [P.sb(st, "pTs", [128, 5, 128], BF16) for _ in range(2)]
            stt = [P.sb(st, "stt", [128, 8], F32) for _ in range(2)]
            osb = P.sb(st, "osb", [128, QHL * 64], BF16)
            oTs = P.sb(st, "oTs", [128, HC, 128], BF16)
            Y = [P.sb(st, "Y", [128, D], F32) for _ in range(2)]
            pp = [P.ps(st, "pp", [128, 512], F32) for _ in range(2)]
            px = P.ps(st, "px", [128, 512], F32)
            psA = P.ps(st, "psA", [128, 512], F32)
            psB = P.ps(st, "psB", [128, 512], F32)
            ppT = P.ps(st, "ppT", [128, 5, 128], BF16)
            po = P.ps(st, "po", [128, 512], F32)
            poT = P.ps(st, "poT", [128, HC, 128], BF16)
            P.op("pe", lambda e: e.matmul(po[:, 0:QHL], CS(C_ONES, 128, 0, 1), skr[0:1, :], start=True, stop=True), R=[cst, skr], W=[po])
            P.op("act", lambda e: e.copy(skb[:, :], po[:, 0:QHL]), R=[po], W=[skb])
            kblk = [(c.SW_Q + h * 128) // 256 for h in range(KVL)]
            koff = [(c.SW_Q + h * 128) % 256 for h in range(KVL)]
            vblk = (c.SW_Q + c.SW_KP) // 256
            blocks = []
            for (q0, n) in groups:
                for h in range(KVL):
                    blocks.append(W_swin[j].blk(kblk[h]))
                blocks.append(W_swin[j].blk(vblk))
            for (q0, n) in groups:
                for hp in range(NHP):
                    blocks.append(W_swin[j].blk(hp // 2))
                for a in range(n):
                    for db in range(ND):
                        blocks.append(W_swout[j].blk(db))
            pf = Pref(ring, blocks, 4)
            bi = 0
            cnt = 0

            def rope_evac(psrc, bias_ap, N, lat, pos0, dst_ap, dst_tk, bias_tk):
                k2 = rope_evac.n % 2
                rope_evac.n += 1
                X, T1 = xf[k2], t1[k2]
                P.op("act", lambda e: e.activation(out=X[:, 0:N], in_=psrc[:, 0:N], func=AF.Identity, bias=bias_ap, scale=1.0), R=[psrc, bias_tk], W=[X])
                if not lat:
                    P.op("dve", lambda e: e.tensor_copy(dst_ap, X[:, 0:N]), R=[X], WP=[dst_tk])
                    return
                P.op("pe", lambda e: e.matmul(px[:, 0:N], CS(C_ROPEP), X[:, 0:N], start=True, stop=True), R=[cst, X], W=[px])
                P.op("dve", lambda e: e.tensor_tensor(T1[:, 0:N], px[:, 0:N], rsn[:, pos0:pos0 + N], ALU.mult), R=[px, rsn], W=[T1])
                P.op("dve", lambda e: e.tensor_tensor(X[:, 0:N], X[:, 0:N], rc[:, pos0:pos0 + N], ALU.mult), R=[rc], W=[X])
                P.op("dve", lambda e: e.tensor_tensor(dst_ap, X[:, 0:N], T1[:, 0:N], ALU.add), R=[X, T1], WP=[dst_tk])
            rope_evac.n = 0

            for gi, (q0, n) in enumerate(groups):
                N = n * 128
                lat = q0 >= c.NCT
                pos0 = (q0 - c.NCT) * 128
                Ug = U[gi % 2]
                for a in range(n):
                    P.dma(ds_ld[gi % 2], Ug[:, :, a * 128:(a + 1) * 128], ucols_of(q0 + a), R=[uT_pair], WP=[Ug])
                for h in range(KVL):
                    slot = pf.get(bi)
                    bi += 1
                    sv = slot.h[:, :].rearrange("p (k c) -> p k c", c=256)
                    pk = pp[cnt % 2]
                    cnt += 1
                    for k in range(KT):
                        P.op("pe", lambda e, k=k: e.matmul(pk[:, 0:N], sv[:, k, koff[h]:koff[h] + 128], Ug[:, k, 0:N], start=(k == 0), stop=(k == KT - 1)),
                             R=[slot, Ug], WP=[pk])
                    rope_evac(pk, bk[:, h:h + 1], N, lat, pos0, KTr[h][:, q0 * 128:q0 * 128 + N], KTr[h], bk)
                slot = pf.get(bi)
                bi += 1
                sv = slot.h[:, :].rearrange("p (k c) -> p k c", c=256)
                for a in range(n):
                    pv = pp[cnt % 2]
                    cnt += 1
                    for k in range(KT):
                        P.op("pe", lambda e, k=k: e.matmul(pv[:, 0:VW], Ug[:, k, a * 128:(a + 1) * 128], sv[:, k, 0:VW], start=(k == 0), stop=False),
                             R=[slot, Ug], WP=[pv])
                    P.op("pe", lambda e: e.matmul(pv[:, 0:VW], ones_bf[0:1, 0:128], bvb[0:1, 0:VW], start=False, stop=True), R=[ones_bf, bvb], WP=[pv])
                    P.op("act", lambda e: e.copy(Vr[:, q0 + a, :], pv[:, 0:VW]), R=[pv], WP=[Vr])

            hc = 0
            yc = 0
            for gi, (q0, n) in enumerate(groups):
                N = n * 128
                lat = q0 >= c.NCT
                pos0 = (q0 - c.NCT) * 128
                Ug = U[gi % 2]
                for a in range(n):
                    P.dma(ds_ld[gi % 2], Ug[:, :, a * 128:(a + 1) * 128], ucols_of(q0 + a), R=[uT_pair], WP=[Ug])
                for hp in range(NHP):
                    slot = pf.get(bi)
                    bi += 1
                    sv = slot.h[:, :].rearrange("p (k c) -> p k c", c=256)
                    pq = pp[cnt % 2]
                    cnt += 1
                    o_ = (hp % 2) * 128
                    for k in range(KT):
                        P.op("pe", lambda e, k=k: e.matmul(pq[:, 0:N], sv[:, k, o_:o_ + 128], Ug[:, k, 0:N], start=(k == 0), stop=(k == KT - 1)),
                             R=[slot, Ug], WP=[pq])
                    rope_evac(pq, bq[:, hp:hp + 1], N, lat, pos0, QT[:, hp, 0:N], QT, bq)
                for a in range(n):
                    q = q0 + a
                    if lat:
                        nl = q - c.NCT
                        lo, hi = max(nl - 1, 0), min(nl + 1, NLT - 1)
                        nb = hi - lo + 1
                        kc0 = CTX + lo * 128
                        mo = C_MASKB + (lo - (nl - 1)) * 128
                        ktiles = list(range(c.NCT)) + [c.NCT + x for x in range(lo, hi + 1)]
                    else:
                        nb = 0
                        ktiles = list(range(c.NCT))
                    nk = CTX + nb * 128
                    nkb = nk // 128
                    for hq in range(QHL):
                        hp, half, kvh = hq // 2, hq % 2, hq // 8
                        p0, p1 = half * 64, half * 64 + 64
                        k2 = hc % 2
                        hc += 1
                        SC, PB, PT, ST = sc[k2], pb[k2], pTs[k2], stt[k2]
                        P.op("pe", lambda e: e.matmul(psA[:, 0:CTX], QT[p0:p1, hp, a * 128:(a + 1) * 128], KTr[kvh][p0:p1, 0:CTX], start=True, stop=True),
                             R=[QT, KTr[kvh]], W=[psA])
                        P.op("act", lambda e: e.copy(SC[:, 0:CTX], psA[:, 0:CTX]), R=[psA], WP=[SC])
                        if nb:
                            P.op("pe", lambda e: e.matmul(psB[:, 0:nb * 128], QT[p0:p1, hp, a * 128:(a + 1) * 128], KTr[kvh][p0:p1, kc0:kc0 + nb * 128], start=True, stop=True),
                                 R=[QT, KTr[kvh]], W=[psB])
                            P.op("dve", lambda e: e.tensor_tensor(SC[:, CTX:nk], psB[:, 0:nb * 128], cst[:, mo:mo + nb * 128], ALU.add), R=[psB, cst], WP=[SC])
                        P.op("dve", lambda e: e.reduce_max(ST[:, 0:1], SC[:, 0:nk], axis=AX.X), R=[SC], WP=[ST])
                        P.op("dve", lambda e: e.tensor_scalar_mul(ST[:, 7:8], ST[:, 0:1], 0.125), R=[ST], WP=[ST])
                        P.op("dve", lambda e: e.tensor_tensor(ST[:, 1:2], ST[:, 7:8], skb[:, hq:hq + 1], ALU.max), R=[ST, skb], WP=[ST])
                        P.op("dve", lambda e: e.tensor_scalar_mul(ST[:, 2:3], ST[:, 1:2], -1.0), R=[ST], WP=[ST])
                        P.op("act", lambda e: e.activation(out=PB[:, 0:nk], in_=SC[:, 0:nk], func=AF.Exp, bias=ST[:, 2:3], scale=0.125), R=[SC, ST], W=[PB])
                        P.op("act", lambda e: e.activation(out=ST[:, 4:5], in_=skb[:, hq:hq + 1], func=AF.Exp, bias=ST[:, 2:3], scale=1.0), R=[skb, ST], WP=[ST])
                        P.op("dve", lambda e: e.reduce_sum(ST[:, 3:4], PB[:, 0:nk], axis=AX.X), R=[PB], WP=[ST])
                        P.op("dve", lambda e: e.tensor_tensor(ST[:, 5:6], ST[:, 3:4], ST[:, 4:5], ALU.add), R=[ST], WP=[ST])
                        P.op("dve", lambda e: e.reciprocal(ST[:, 6:7], ST[:, 5:6]), R=[ST], WP=[ST])
                        for kb in range(nkb):
                            P.op("pe", lambda e, kb=kb: e.transpose(ppT[:, kb, :], PB[:, kb * 128:(kb + 1) * 128], ident_bf[:, :]), R=[PB, ident_bf], WP=[ppT])
                        P.op("act", lambda e: e.copy(PT[:, 0:nkb, :], ppT[:, 0:nkb, :]), R=[ppT], W=[PT])
                        oreg = po[:, (hq % 8) * 64:(hq % 8) * 64 + 64]
                        for kb in range(nkb):
                            P.op("pe", lambda e, kb=kb: e.matmul(oreg, PT[:, kb, :], Vr[:, ktiles[kb], kvh * 64:(kvh + 1) * 64], start=(kb == 0), stop=(kb == nkb - 1)),
                                 R=[PT, Vr], WP=[po])
                        P.op("dve", lambda e: e.tensor_scalar_mul(osb[:, hq * 64:(hq + 1) * 64], oreg, ST[:, 6:7]), R=[po, ST], WP=[osb])
                    for kc in range(HC):
                        P.op("pe", lambda e, kc=kc: e.transpose(poT[:, kc, :], osb[:, kc * 128:(kc + 1) * 128], ident_bf[:, :]), R=[osb, ident_bf], WP=[poT])
                    P.op("act", lambda e: e.copy(oTs[:, :, :], poT[:, :, :]), R=[poT], W=[oTs])
                    rk, lt = seq_tile(q)
                    Yt = Y[yc % 2]
                    yc += 1
                    for db in range(ND):
                        slot = pf.get(bi)
                        bi += 1
                        so = slot.h[:, 0:HC * 512].rearrange("p (k c) -> p k c", c=512)
                        pq = pp[cnt % 2]
                        cnt += 1
                        for kc in range(HC):
                            P.op("pe", lambda e, kc=kc: e.matmul(pq[:, :], oTs[:, kc, :], so[:, kc, :], start=(kc == 0), stop=False), R=[oTs, slot], WP=[pq])
                        P.op("pe", lambda e: e.matmul(pq[:, :], CS(C_HALF, 128, 0, 1), bor[0:1, db * 512:(db + 1) * 512], start=False, stop=True), R=[cst, bor], WP=[pq])
                        P.op("act", lambda e: e.copy(Yt[:, db * 512:(db + 1) * 512], pq[:, :]), R=[pq], WP=[Yt])
                    r0 = (rk * NT + lt) * 128
                    P.dma(ds_st[yc % 2], yp.h[r0:r0 + 128, :], Yt[:, :], R=[Yt], WP=[yp], q="act")
            P.barrier()

    def gla_stage(i, j):
        DK, DV, DKC, DVC = c.DK, c.DV, c.DKC, c.DVC
        QC = 2 * DKC
        GC = 2 * DVC
        W2 = 2 * DK
        nqb = (2 * DK) // 256
        nvb = (2 * DV) // 256
        base_groups = seq_groups()
        ctx_g = [g for g in base_groups if g[0] < c.NCT]
        lat_g = [g for g in base_groups if g[0] >= c.NCT]
        with ExitStack() as st:
            ring = Ring(P, st, 5, "ringg")
            w1f = P.sb(st, "w1f", [128, KT, c.RANK], F32)
            w1b = [P.sb(st, "w1b", [128, KT, c.RANK], BF16) for _ in range(2)]
            w2 = [P.sb(st, "w2", [c.RANK, W2], F32) for _ in range(2)]
            gb = [P.sb(st, "gb", [1, W2], F32) for _ in range(2)]
            ng = P.sb(st, "ng", [128, DVC], F32)
            P.dma(ds_misc, ng[:, :], gla_ng_in.h[j].rearrange("(cc p) -> p cc", p=128), R=[gla_ng_in], W=[ng], slow=True)
            for d in range(2):
                P.dma(ds_misc, w1f[:, :, :], gla_w1_in.h[j, d].rearrange("(k p) r -> p k r", p=128), R=[gla_w1_in], W=[w1f])
                P.op("dve", lambda e, d=d: e.tensor_copy(w1b[d][:, :, :], w1f[:, :, :]), R=[w1f], W=[w1b[d]])
                P.dma(ds_misc, w2[d][:, :], gla_w2_in.h[j, d], R=[gla_w2_in], W=[w2[d]])
                P.dma(ds_misc, gb[d][:, :], gla_gb_in[j, d:d + 1, :], R=[gla_gb_in], W=[gb[d]])
            Ug = P.sb(st, "Ug", [128, KT, 512], BF16)
            qT = P.sb(st, "qT", [128, QC, 512], F32)
            kT = P.sb(st, "kT", [128, QC, 512], F32)
            ktm = P.sb(st, "ktm", [128, 4, W2], F32)
            vtm = P.sb(st, "vtm", [128, 4, 2 * DV], BF16)
            gT = P.sb(st, "gT", [128, GC, 512], F32)
            zr = P.sb(st, "zr", [c.RANK, 512], F32)
            la0 = P.sb(st, "la0", [128, W2], F32)
            la = P.sb(st, "la", [128, W2], F32)
            E1 = P.sb(st, "E1", [128, QC, 128], F32)
            E2 = P.sb(st, "E2", [128, QC, 128], F32)
            E3 = P.sb(st, "E3", [128, W2], F32)
            qd = P.sb(st, "qd", [128, QC, 128], BF16)
            ki = P.sb(st, "ki", [128, QC, 128], BF16)
            ks = P.sb(st, "ks", [128, W2], BF16)
            scT = [P.sb(st, "scT", [128, 128], BF16) for _ in range(2)]
            S32 = [P.sb(st, "S32", [128, DKC, DV], F32) for _ in range(2)]
            Sbf = [P.sb(st, "Sbf", [128, DKC, DV], BF16) for _ in range(2)]
            osb = P.sb(st, "osb", [128, GC, 128], F32)
            ofw = P.sb(st, "ofw", [128, GC, 128], F32)
            osq = P.sb(st, "osq", [128, GC, 128], F32)
            rstd = [P.sb(st, "rstd", [128, 128], F32) for _ in range(2)]
            ofin = P.sb(st, "ofin", [128, GC, 128], BF16)
            Y = P.sb(st, "Y", [128, D], F32)
            pA = [P.ps(st, "pA", [128, 512], F32) for _ in range(2)]
            pB = P.ps(st, "pB", [128, 512], F32)
            pC = P.ps(st, "pC", [128, 512], F32)
            pD = P.ps(st, "pD", [128, 512], F32)
            pO = [P.ps(st, "pO", [128, DVC, 128], F32) for _ in range(2)]
            pS = P.ps(st, "pS", [128, 512], F32)
            ofk = [Tk(None, "ofwd%d" % q) for q in range(c.NTL)]
            cnt = 0
            for d in range(2):
                tri_i = CS(C_TFI) if d == 0 else CS(C_TBI)
                tri_e = CS(C_TFE) if d == 0 else CS(C_TBE)
                mbf = mask_f_bf if d == 0 else mask_b_bf
                groups = (ctx_g + lat_g) if d == 0 else (ctx_g[::-1] + lat_g[::-1])
                for h in range(2):
                    P.op("dve", lambda e, h=h: e.memset(S32[h][:, :, :], 0.0), W=[S32[h]])
                    P.op("dve", lambda e, h=h: e.memset(Sbf[h][:, :, :], 0.0), W=[Sbf[h]])
                blocks = []
                for (q0, n) in groups:
                    nb_ = 2 * nqb + nvb + (nvb if d == 1 else 0)
                    for b_ in range(nb_):
                        blocks.append(W_glin[j].blk(b_ if b_ < 2 * nqb + nvb else b_))
                    if d == 1:
                        for a in range(n):
                            for db in range(ND):
                                blocks.append(W_glout[j].blk(db))
                pf = Pref(ring, blocks, 3)
                bi = 0
                for (q0, n) in groups:
                    N = n * 128
                    for a in range(n):
                        P.dma(ds_ld[0], Ug[:, :, a * 128:(a + 1) * 128], ucols_of(q0 + a), R=[uT_pair], WP=[Ug])
                    pz = pA[cnt % 2]
                    cnt += 1
                    for k in range(KT):
                        P.op("pe", lambda e, k=k: e.matmul(pz[0:c.RANK, 0:N], w1b[d][:, k, :], Ug[:, k, 0:N], start=(k == 0), stop=(k == KT - 1)),
                             R=[w1b[d], Ug], WP=[pz])
                    P.op("act", lambda e: e.copy(zr[:, 0:N], pz[0:c.RANK, 0:N]), R=[pz], W=[zr])
                    for part, dstT in ((0, qT), (1, kT)):
                        for b_ in range(nqb):
                            slot = pf.get(bi)
                            bi += 1
                            sv = slot.h[:, :].rearrange("p (k c) -> p k c", c=256)
                            for h2 in range(2):
                                qc = b_ * 2 + h2
                                pq = pA[cnt % 2]
                                cnt += 1
                                for k in range(KT):
                                    P.op("pe", lambda e, k=k: e.matmul(pq[:, 0:N], sv[:, k, h2 * 128:(h2 + 1) * 128], Ug[:, k, 0:N], start=(k == 0), stop=(k == KT - 1)),
                                         R=[slot, Ug], WP=[pq])
                                sc_ = float(DK) ** -0.5 if part == 0 else 1.0
                                P.op("act", lambda e: e.activation(out=dstT[:, qc, 0:N], in_=pq[:, 0:N], func=AF.Identity, scale=sc_), R=[pq], WP=[dstT])
                            if part == 1:
                                for a in range(n):
                                    pk = pA[cnt % 2]
                                    cnt += 1
                                    for k in range(KT):
                                        P.op("pe", lambda e, k=k: e.matmul(pk[:, 0:256], Ug[:, k, a * 128:(a + 1) * 128], sv[:, k, :], start=(k == 0), stop=(k == KT - 1)),
                                             R=[slot, Ug], WP=[pk])
                                    P.op("act", lambda e: e.copy(ktm[:, a, b_ * 256:(b_ + 1) * 256], pk[:, 0:256]), R=[pk], WP=[ktm])
                    for b_ in range(nvb):
                        slot = pf.get(bi)
                        bi += 1
                        sv = slot.h[:, :].rearrange("p (k c) -> p k c", c=256)
                        for a in range(n):
                            pv = pA[cnt % 2]
                            cnt += 1
                            for k in range(KT):
                                P.op("pe", lambda e, k=k: e.matmul(pv[:, 0:256], Ug[:, k, a * 128:(a + 1) * 128], sv[:, k, :], start=(k == 0), stop=(k == KT - 1)),
                                     R=[slot, Ug], WP=[pv])
                            P.op("act", lambda e: e.copy(vtm[:, a, b_ * 256:(b_ + 1) * 256], pv[:, 0:256]), R=[pv], WP=[vtm])
                    if d == 1:
                        for b_ in range(nvb):
                            slot = pf.get(bi)
                            bi += 1
                            sv = slot.h[:, :].rearrange("p (k c) -> p k c", c=256)
                            for h2 in range(2):
                                gc = b_ * 2 + h2
                                pq = pA[cnt % 2]
                                cnt += 1
                                for k in range(KT):
                                    P.op("pe", lambda e, k=k: e.matmul(pq[:, 0:N], sv[:, k, h2 * 128:(h2 + 1) * 128], Ug[:, k, 0:N], start=(k == 0), stop=(k == KT - 1)),
                                         R=[slot, Ug], WP=[pq])
                                P.op("act", lambda e: e.activation(out=gT[:, gc, 0:N], in_=pq[:, 0:N], func=AF.Silu), R=[pq], WP=[gT])
                    order = list(range(n)) if d == 0 else list(range(n - 1, -1, -1))
                    for a in order:
                        q = q0 + a
                        ac = slice(a * 128, (a + 1) * 128)
                        P.op("pe", lambda e: e.matmul(pB[:, 0:W2], zr[0:c.RANK, ac], w2[d][:, :], start=True, stop=False), R=[zr, w2[d]], WP=[pB])
                        P.op("pe", lambda e: e.matmul(pB[:, 0:W2], CS(C_ONES, 128, 0, 1), gb[d][0:1, :], start=False, stop=True), R=[cst, gb[d]], WP=[pB])
                        P.op("act", lambda e: e.activation(out=la0[:, :], in_=pB[:, 0:W2], func=AF.Exp, scale=-1.0), R=[pB], W=[la0])
                        P.op("act", lambda e: e.activation(out=la[:, :], in_=la0[:, :], func=AF.Ln, bias=1.0, scale=1.0), R=[la0], W=[la])
                        for qc in range(QC):
                            P.op("pe", lambda e, qc=qc: e.matmul(pC[:, qc * 128:(qc + 1) * 128], la[:, qc * 128:(qc + 1) * 128], tri_i, start=(qc == 0), stop=(qc == QC - 1)),
                                 R=[la, cst], WP=[pC])
                        pCv = pC[:, 0:QC * 128].rearrange("p (q c) -> p q c", c=128)
                        P.op("act", lambda e: e.activation(out=E1[:, :, :], in_=pCv, func=AF.Exp, scale=-1.0 / 16), R=[pC], W=[E1])
                        P.op("act", lambda e: e.activation(out=E2[:, :, :], in_=pCv, func=AF.Exp, scale=1.0 / 16), R=[pC], W=[E2])
                        P.op("dve", lambda e: e.tensor_tensor(qd[:, :, :], qT[:, :, ac], E1[:, :, :], ALU.mult), R=[qT, E1], W=[qd])
                        P.op("dve", lambda e: e.tensor_tensor(ki[:, :, :], kT[:, :, ac], E2[:, :, :], ALU.mult), R=[kT, E2], W=[ki])
                        P.op("pe", lambda e: e.matmul(pB[:, 0:W2], tri_e, la[:, :], start=True, stop=True), R=[cst, la], WP=[pB])
                        P.op("act", lambda e: e.activation(out=E3[:, :], in_=pB[:, 0:W2], func=AF.Exp, scale=-1.0 / 16), R=[pB], W=[E3])
                        P.op("dve", lambda e: e.tensor_tensor(ks[:, :], ktm[:, a, :], E3[:, :], ALU.mult), R=[ktm, E3], W=[ks])
                        for h in range(2):
                            for dc in range(DKC):
                                P.op("pe", lambda e, dc=dc: e.matmul(pD[:, h * 128:(h + 1) * 128], ki[:, h * DKC + dc, :], qd[:, h * DKC + dc, :],
                                                                     start=(h == 0 and dc == 0), stop=(dc == DKC - 1)),
                                     R=[ki, qd], WP=[pD])
                        for h in range(2):
                            P.op("dve", lambda e, h=h: e.tensor_tensor(scT[h][:, :], pD[:, h * 128:(h + 1) * 128], mbf[:, :], ALU.mult), R=[pD, mbf], W=[scT[h]])
                        for h in range(2):
                            for ec in range(DVC):
                                P.op("pe", lambda e, ec=ec: e.matmul(pO[h][:, ec, :], vtm[:, a, h * DV + ec * 128:h * DV + (ec + 1) * 128], scT[h][:, :],
                                                                     start=(ec == 0), stop=False),
                                     R=[vtm, scT[h]], WP=[pO[h]])
                        chunks = (0, 1) if d == 0 else (1, 0)
                        for ci, ch in enumerate(chunks):
                            cs_ = slice(ch * 64, (ch + 1) * 64)
                            for h in range(2):
                                for ec in range(DVC):
                                    for dc in range(DKC):
                                        P.op("pe", lambda e, ec=ec, dc=dc: e.matmul(pO[h][:, ec, cs_], Sbf[h][:, dc, ec * 128:(ec + 1) * 128], qd[:, h * DKC + dc, cs_],
                                                                                    start=False, stop=(ci == 1 and dc == DKC - 1)),
                                             R=[Sbf[h], qd], WP=[pO[h]])
                            col = ch * 64 + 63 if d == 0 else ch * 64
                            for h in range(2):
                                for dc in range(DKC):
                                    P.op("pe", lambda e, dc=dc: e.matmul(pS[:, 0:DV], ks[cs_, h * DK + dc * 128:h * DK + (dc + 1) * 128], vtm[cs_, a, h * DV:(h + 1) * DV],
                                                                         start=True, stop=True),
                                         R=[ks, vtm], W=[pS])
                                    P.op("dve", lambda e, dc=dc: e.scalar_tensor_tensor(S32[h][:, dc, :], S32[h][:, dc, :], E1[:, h * DKC + dc, col:col + 1], pS[:, 0:DV],
                                                                                        ALU.mult, ALU.add),
                                         R=[pS, E1], W=[S32[h]])
                                P.op("act", lambda e, h=h: e.copy(Sbf[h][:, :, :], S32[h][:, :, :]), R=[S32[h]], W=[Sbf[h]])
                        r0q = q * 128
                        if d == 0:
                            for h in range(2):
                                P.op("act", lambda e, h=h: e.copy(osb[:, h * DVC:(h + 1) * DVC, :], pO[h][:, :, :]), R=[pO[h]], WP=[osb])
                            P.dma(ds_st[0], ofwd.h[r0q:r0q + 128, :].rearrange("p (g c) -> p g c", c=128), osb[:, :, :], R=[osb], W=[ofk[q]], q="act")
                            continue
                        P.dma(ds_ld[1], ofw[:, :, :], ofwd.h[r0q:r0q + 128, :].rearrange("p (g c) -> p g c", c=128), R=[ofk[q]], W=[ofw])
                        for h in range(2):
                            P.op("dve", lambda e, h=h: e.tensor_tensor(osb[:, h * DVC:(h + 1) * DVC, :], pO[h][:, :, :], ofw[:, h * DVC:(h + 1) * DVC, :], ALU.add),
                                 R=[pO[h], ofw], WP=[osb])
                        P.op("act", lambda e: e.activation(out=osq[:, :, :], in_=osb[:, :, :], func=AF.Square), R=[osb], W=[osq])
                        for h in range(2):
                            for ec in range(DVC):
                                P.op("pe", lambda e, ec=ec: e.matmul(pC[:, h * 128:(h + 1) * 128], CS(C_ONES), osq[:, h * DVC + ec, :], start=(h == 0 and ec == 0), stop=(ec == DVC - 1)),
                                     R=[cst, osq], WP=[pC])
                        for h in range(2):
                            P.op("dve", lambda e, h=h: e.tensor_scalar(rstd[h][:, :], pC[:, h * 128:(h + 1) * 128], 1.0 / DV, LN_EPS, ALU.mult, ALU.add), R=[pC], W=[rstd[h]])
                            P.op("act", lambda e, h=h: e.sqrt(rstd[h][:, :], rstd[h][:, :]), W=[rstd[h]])
                            P.op("dve", lambda e, h=h: e.reciprocal(rstd[h][:, :], rstd[h][:, :]), W=[rstd[h]])
                            for ec in range(DVC):
                                gcx = h * DVC + ec
                                P.op("dve", lambda e, gcx=gcx: e.tensor_tensor(osb[:, gcx, :], osb[:, gcx, :], rstd[h][:, :], ALU.mult), R=[rstd[h]], W=[osb])
                                P.op("dve", lambda e, gcx=gcx, ec=ec: e.scalar_tensor_tensor(ofin[:, gcx, :], osb[:, gcx, :], ng[:, ec:ec + 1], gT[:, gcx, ac], ALU.mult, ALU.mult),
                                     R=[osb, ng, gT], WP=[ofin])
                        rk, lt = seq_tile(q)
                        for db in range(ND):
                            slot = pf.get(bi)
                            bi += 1
                            so = slot.h[:, 0:GC * 512].rearrange("p (k c) -> p k c", c=512)
                            pq = pA[cnt % 2]
                            cnt += 1
                            for kc in range(GC):
                                P.op("pe", lambda e, kc=kc: e.matmul(pq[:, :], ofin[:, kc, :], so[:, kc, :], start=(kc == 0), stop=(kc == GC - 1)), R=[ofin, slot], WP=[pq])
                            P.op("act", lambda e: e.copy(Y[:, db * 512:(db + 1) * 512], pq[:, :]), R=[pq], WP=[Y])
                        r0 = (rk * NT + lt) * 128
                        P.dma(ds_st[1], yp.h[r0:r0 + 128, :], Y[:, :], R=[Y], WP=[yp], q="act")
                P.barrier()

    def conv_stage(i, j):
        HC = c.HC
        nin = (D // 2) // 256
        wins = []
        for (a0, b0) in ((0, c.NCT), (c.NCT, c.NTL)):
            q = a0
            while q < b0:
                n = min(3, b0 - q)
                wins.append((q, n, q == a0, q + n == b0))
                q += n
        with ExitStack() as st:
            ring = Ring(P, st, 6, "ringc")
            cw = P.sb(st, "cw", [128, 3, HC], F32)
            P.dma(ds_misc, cw[:, :, :], conv_w_in.h[j].rearrange("t (cc p) -> p t cc", p=128), R=[conv_w_in], W=[cw], slow=True)
            uw = [P.sb(st, "uw", [128, KT, 386], BF16) for _ in range(2)]
            gi_sb = [P.sb(st, "gi_sb", [128, 386], F32) for _ in range(2)]
            p_sb = [P.sb(st, "p_sb", [128, 386], F32) for _ in range(2)]
            z_sb = [P.sb(st, "z_sb", [128, 384], F32) for _ in range(2)]
            qT = [P.sb(st, "qT", [128, HC, 384], BF16) for _ in range(2)]
            ytile = [P.sb(st, "ytile", [128, D], F32) for _ in range(2)]
            pgi = [P.ps(st, "pgi", [128, 512], F32) for _ in range(2)]
            pva = [P.ps(st, "pva", [128, 512], F32) for _ in range(2)]
            pgo = [P.ps(st, "pgo", [128, 512], F32) for _ in range(2)]
            po = [P.ps(st, "po", [128, 512], F32) for _ in range(2)]
            blocks = []
            for (q0, n, s0, s1) in wins:
                for cp in range(HC // 2):
                    blocks += [W_cvin[j].blk(cp), W_cvin[j].blk(nin + cp), W_cvin[j].blk(2 * nin + cp)]
                for a in range(n):
                    for db in range(ND):
                        blocks.append(W_cvout[j].blk(db))
            pf = Pref(ring, blocks, 3)
            bi = 0
            cnt = 0
            yc = 0
            for wi, (q0, n, s0, s1) in enumerate(wins):
                N = n * 128 + 2
                U = uw[wi % 2]
                Q = qT[wi % 2]

                def ucols(q):
                    rk, lt = seq_tile(q)
                    return uT_pair_d[rk].h[lt * 128:(lt + 1) * 128, :].rearrange("p (k c) -> p k c", c=128)
                for a in range(n):
                    P.dma(ds_ld[wi % 2], U[:, :, 1 + a * 128:1 + (a + 1) * 128], ucols(q0 + a), R=[uT_pair], WP=[U])
                if s0:
                    P.op("dve", lambda e: e.memset(U[:, :, 0:1], 0.0), WP=[U])
                else:
                    P.dma(ds_ld[wi % 2], U[:, :, 0:1], ucols(q0 - 1)[:, :, 127:128], R=[uT_pair], WP=[U], slow=True)
                if s1:
                    P.op("dve", lambda e: e.memset(U[:, :, N - 1:N], 0.0), WP=[U])
                else:
                    P.dma(ds_ld[wi % 2], U[:, :, N - 1:N], ucols(q0 + n)[:, :, 0:1], R=[uT_pair], WP=[U], slow=True)
                for cp in range(HC // 2):
                    sl = [pf.get(bi), pf.get(bi + 1), pf.get(bi + 2)]
                    bi += 3
                    sv = [x.h[:, :].rearrange("p (k c) -> p k c", c=256) for x in sl]
                    for h2 in range(2):
                        cc = cp * 2 + h2
                        k2 = cnt % 2
                        cnt += 1
                        PGI, PGO, PVA, GI, PS, ZS = pgi[k2], pgo[k2], pva[k2], gi_sb[k2], p_sb[k2], z_sb[k2]
                        for (pt, si) in ((PGI, 0), (PVA, 2), (PGO, 1)):
                            for k in range(KT):
                                P.op("pe", lambda e, k=k, pt=pt, si=si: e.matmul(pt[:, 0:N], sv[si][:, k, h2 * 128:(h2 + 1) * 128], U[:, k, 0:N],
                                                                                 start=(k == 0), stop=(k == KT - 1)),
                                     R=[sl[si], U], WP=[pt])
                        P.op("act", lambda e: e.copy(GI[:, 0:N], PGI[:, 0:N]), R=[PGI], W=[GI])
                        P.op("dve", lambda e: e.tensor_tensor(PS[:, 0:N], PVA[:, 0:N], GI[:, 0:N], ALU.mult), R=[PVA, GI], W=[PS])
                        P.op("dve", lambda e: e.tensor_scalar_mul(ZS[:, 0:N - 2], PS[:, 1:N - 1], cw[:, 1, cc:cc + 1]), R=[PS, cw], W=[ZS])
                        P.op("dve", lambda e: e.scalar_tensor_tensor(ZS[:, 0:N - 2], PS[:, 0:N - 2], cw[:, 0, cc:cc + 1], ZS[:, 0:N - 2], ALU.mult, ALU.add),
                             R=[PS, cw], W=[ZS])
                        P.op("dve", lambda e: e.scalar_tensor_tensor(ZS[:, 0:N - 2], PS[:, 2:N], cw[:, 2, cc:cc + 1], ZS[:, 0:N - 2], ALU.mult, ALU.add),
                             R=[PS, cw], W=[ZS])
                        P.op("dve", lambda e: e.tensor_tensor(Q[:, cc, 0:N - 2], PGO[:, 1:N - 1], ZS[:, 0:N - 2], ALU.mult), R=[PGO, ZS], WP=[Q])
                for a in range(n):
                    rk, lt = seq_tile(q0 + a)
                    Y = ytile[yc % 2]
                    yc += 1
                    for db in range(ND):
                        slot = pf.get(bi)
                        bi += 1
                        so = slot.h[:, 0:HC * 512].rearrange("p (k c) -> p k c", c=512)
                        pp = po[db % 2]
                        for cc in range(HC):
                            P.op("pe", lambda e, cc=cc: e.matmul(pp[:, :], Q[:, cc, a * 128:(a + 1) * 128], so[:, cc, :], start=(cc == 0), stop=(cc == HC - 1)),
                                 R=[Q, slot], WP=[pp])
                        P.op("act", lambda e: e.copy(Y[:, db * 512:(db + 1) * 512], pp[:, :]), R=[pp], WP=[Y])
                    r0 = (rk * NT + lt) * 128
                    P.dma(ds_st[yc % 2], yp.h[r0:r0 + 128, :], Y[:, :], R=[Y], WP=[yp], q="act")
            P.barrier()

    def zero_y():
        with ExitStack() as st:
            z = P.sb(st, "zero", [128, D], F32)
            P.op("dve", lambda e: e.memset(z[:, :], 0.0), W=[z])
            for t in range(NT):
                P.dma(ds_st[t % 2], y_own[t * 128:(t + 1) * 128, :], z[:, :], R=[z], WP=[y_own], q="act")
            P.barrier()

    mixers = {}
    if mixers_on[0]:
        mixers[0] = gla_stage
    if mixers_on[1]:
        mixers[1] = swa_stage
    if mixers_on[2]:
        mixers[2] = conv_stage
    token_stage(0, 0, first=True)
    if cut == "pre":
        return finish()
    if cut == "pre_dump":
        for t in range(NT):
            P.dma(ds_st[t % 2], out_t[t * 128:(t + 1) * 128, :], h_tiles[t][:, :], R=[h_tiles[t]], WP=[out_t], q="act")
        return finish()
    if cut == "pre_zero":
        zero_y()
        return finish()
    nsub = 0
    for i in range(DEPTH):
        kind, j = mixer_kind(i)
        if kind in mixers:
            for m_ in range(2):
                P.coll(cs_a, "ReduceScatter", ALU.add, GPAIR, xbuf_d[m_].h.ap(), uT_pair_d[m_].h.ap(), R=[xbuf], W=[uT_pair])
            mixers[kind](i, j)
            P.coll(cs_a, "ReduceScatter", ALU.add, GPAIR, yp.h.ap(), y_own.h.ap(), R=[yp], W=[y_own])
        else:
            zero_y()
        token_stage(i, 0)
        nsub += 1
        if stop is not None and nsub >= stop:
            break
        for m_ in range(2):
            P.coll(cs_a, "ReduceScatter", ALU.add, GPAIR, xbuf_d[m_].h.ap(), uT_pair_d[m_].h.ap(), R=[xbuf], W=[uT_pair])
            P.coll(cs_a, "ReduceScatter", ALU.add, GPAIR, gxbuf_d[m_].h.ap(), gpair_d[m_].h.ap(), R=[gxbuf], W=[gpair])
        moe_stage(i)
        P.coll(cs_a, "ReduceScatter", ALU.add, GPAIR, yp.h.ap(), y_own.h.ap(), R=[yp], W=[y_own])
        token_stage(i, 1)
        nsub += 1
        if stop is not None and nsub >= stop:
            break
    if stop is not None:
        for t in range(NT):
            P.dma(ds_st[t % 2], out_t[t * 128:(t + 1) * 128, :], h_tiles[t][:, :], R=[h_tiles[t]], WP=[out_t], q="act")
    P.final_wait()
    pst.close()
    return nc


def shard_inputs(cfg, inp):
    c = cfg
    D = c.D
    f = lambda a: np.ascontiguousarray(np.asarray(a, dtype=np.float32))
    consts = make_consts()
    rope = make_rope(c)
    EL = c.E // 2
    maps = []
    for cid in range(8):
        b, r = cid // 2, cid % 2
        m = {}
        m["h_in"] = f(np.concatenate([inp["ctx"][b, r * c.CH:(r + 1) * c.CH], inp["x"][b, r * c.LH:(r + 1) * c.LH]], 0))
        call = np.stack([np.asarray(inp["c"])[b], np.asarray(inp["c_ctx"])], 0)
        m["cond"] = f(call[:, r * (D // 2):(r + 1) * (D // 2)])
        m["consts"] = consts
        fl = np.zeros((128, 10), np.float32)
        fl[:, r] = 1.0
        fl[:, 2 + cid] = 1.0
        m["flags"] = fl
        m["rope"] = rope
        m["ada_w"] = f(inp["ada_w"][:, r * (D // 2):(r + 1) * (D // 2), :])
        m["ada_b"] = f(inp["ada_b"])
        m["ln_g"] = f(inp["ln_g"])
        m["ln_b"] = f(inp["ln_b"])
        m["moe_gu"] = f(inp["moe_w_gate_up"][:, r * EL:(r + 1) * EL])
        m["moe_d"] = f(inp["moe_w_down"][:, r * EL:(r + 1) * EL])
        m["moe_rw"] = f(inp["moe_router_w"])
        m["moe_rb"] = f(inp["moe_router_b"])
        m["moe_bgu"] = f(np.asarray(inp["moe_b_gate_up"])[:, r * EL:(r + 1) * EL])
        m["moe_bd"] = f(np.asarray(inp["moe_b_down"])[:, r * EL:(r + 1) * EL])
        rs = slice(None)
        KW, VW, DK, DV = c.KW, D, c.DK, c.DV
        w = np.asarray(inp["gla_w_in"])
        cols = np.concatenate([np.arange(r * 2 * DK, (r + 1) * 2 * DK), KW + np.arange(r * 2 * DK, (r + 1) * 2 * DK),
                               2 * KW + np.arange(r * 2 * DV, (r + 1) * 2 * DV), 2 * KW + VW + np.arange(r * 2 * DV, (r + 1) * 2 * DV)])
        m["gla_win"] = f(w[:, rs][:, :, cols])
        m["gla_w1"] = f(inp["gla_gate_w1"])
        m["gla_w2"] = f(np.asarray(inp["gla_gate_w2"])[:, :, :, r * 2 * DK:(r + 1) * 2 * DK])
        m["gla_gb"] = f(np.asarray(inp["gla_gate_b"])[:, :, r * 2 * DK:(r + 1) * 2 * DK])
        m["gla_ng"] = f(inp["gla_norm_g"])
        wo = np.asarray(inp["gla_w_out"])[:, r * 2 * DV:(r + 1) * 2 * DV]
        m["gla_wout"] = f(wo)
        QW = c.QH * 64
        KVW = c.KVH * 64
        w = np.asarray(inp["swa_w_qkv"])
        bq = np.asarray(inp["swa_b_qkv"])
        qcols = np.arange(r * c.SW_Q, (r + 1) * c.SW_Q)
        kcols = np.concatenate([np.tile(QW + (r * c.KVL + hh) * 64 + np.arange(64), 2) for hh in range(c.KVL)])
        vcols = np.concatenate([QW + KVW + (r * c.KVL + hh) * 64 + np.arange(64) for hh in range(c.KVL)])
        ws = w[:, rs]
        win = np.zeros((w.shape[0], D, c.SW_COLS), np.float32)
        win[:, :, 0:c.SW_Q] = ws[:, :, qcols]
        win[:, :, c.SW_Q:c.SW_Q + c.SW_K] = ws[:, :, kcols]
        win[:, :, c.SW_Q + c.SW_KP:c.SW_Q + c.SW_KP + c.SW_V] = ws[:, :, vcols]
        m["swa_win"] = win
        m["swa_bq"] = f(bq[:, qcols])
        m["swa_bk"] = f(bq[:, kcols])
        bv = np.zeros((w.shape[0], 128), np.float32)
        bv[:, 0:c.SW_V] = bq[:, vcols]
        m["swa_bv"] = bv
        m["swa_sink"] = f(np.asarray(inp["swa_sink"])[:, r * c.QHL:(r + 1) * c.QHL])
        wo = np.asarray(inp["swa_w_out"])[:, r * (D // 2):(r + 1) * (D // 2)]
        m["swa_wout"] = f(wo)
        m["swa_bout"] = f(inp["swa_b_out"])
        if c.NC > 0:
            w = np.asarray(inp["conv_w_in"])
            hcols = np.arange(r * (D // 2), (r + 1) * (D // 2))
            cols = np.concatenate([hcols, D + hcols, 2 * D + hcols])
            m["conv_win"] = f(w[:, rs][:, :, cols])
            m["conv_w"] = f(np.asarray(inp["conv_w"])[:, :, hcols])
            wo = np.asarray(inp["conv_w_out"])[:, r * (D // 2):(r + 1) * (D // 2)]
            m["conv_wout"] = f(wo)
        else:
            m["conv_win"] = np.random.RandomState(0).randn(1, D, 3 * D // 2).astype(np.float32)
            m["conv_w"] = np.random.RandomState(1).randn(1, 3, D // 2).astype(np.float32)
            m["conv_wout"] = np.random.RandomState(2).randn(1, D // 2, D).astype(np.float32)
        maps.append(m)
    return maps


def run(cfg, inp, stop=None, mixers_on=(True, True, True), dbg=False, cut=None):
    nc = build(cfg, stop=stop, mixers_on=mixers_on, dbg=dbg, cut=cut)
    maps = shard_inputs(cfg, inp)
    res = run_bass_kernel_spmd(nc, maps, core_ids=list(range(8)))
    outs = [np.asarray(res.results[i]["out"]) for i in range(8)]
    return outs


def assemble(cfg, outs):
    c = cfg
    y = np.zeros((c.B, c.S, c.D), np.float32)
    for cid in range(8):
        b, r = cid // 2, cid % 2
        y[b, r * c.LH:(r + 1) * c.LH] = outs[cid][c.CH:]
    return y


def kernel(**inputs):
    cfg = Cfg()
    outs = run(cfg, inputs)
    return assemble(cfg, outs)
```
